# Optimizing a Trainium2 kernel written in Bass

```python
import math
import jax, jax.numpy as jnp
from jax import lax
import numpy as np

D_MODEL = 1024
BATCH = 1
SEQ = 16384
DEPTH = 4

GRID_W = 64
CTX_LEN = 256
RWKV_WIDTH = D_MODEL // 2
HEAD_DIM = 64
RWKV_HEADS = RWKV_WIDTH // HEAD_DIM
LORA_W = 64
LORA_A = 64
LORA_G = 128
DECAY_SCALE = 0.606531
GN_EPS = 6.4e-4
RWKV_COLS = 3 * RWKV_WIDTH + 2 * LORA_W + 2 * LORA_A + LORA_G
RWKV_SPLITS = (RWKV_WIDTH, 2 * RWKV_WIDTH, 3 * RWKV_WIDTH, 3 * RWKV_WIDTH + LORA_W, 3 * RWKV_WIDTH + 2 * LORA_W,
               3 * RWKV_WIDTH + 2 * LORA_W + LORA_A, 3 * RWKV_WIDTH + 2 * LORA_W + 2 * LORA_A)
HYENA_WIDTH = D_MODEL // 2
HYENA_COLS = 3 * HYENA_WIDTH
FILTER_BANDS = 16
FILTER_EMB = 1 + 2 * FILTER_BANDS
FILTER_HIDDEN = 64
HYENA_MIN_DECAY = math.log(1e-2) / 1.5
HYENA_MAX_DECAY = math.log(1e-2) / 0.3
GATE_COLS = 2 * D_MODEL
PROJ_COLS = RWKV_COLS + HYENA_COLS + GATE_COLS
N_EXPERTS = 16
N_GROUPS = 4
EXPERTS_PER_GROUP = N_EXPERTS // N_GROUPS
TOP_K = 2
D_EXPERT = D_MODEL
MOE_BLOCK = 128
ALPHA = (2 * DEPTH) ** 0.25
BETA = (8 * DEPTH) ** -0.25
LN_EPS = 1e-5

kernel_name = 'hybrid_rwkv7_hyena_moe_diffusion'


def _shift_seq(t, offset):
    if offset > 0:
        return jnp.pad(t[:, :-1], ((0, 0), (1, 0), (0, 0)))
    return jnp.pad(t[:, 1:], ((0, 0), (0, 1), (0, 0)))


def token_shift_latent(p):
    b, n, ch = p.shape
    rows = n // GRID_W
    q = ch // 4
    g = p.reshape(b, rows, GRID_W, ch)
    left = jnp.pad(g[:, :, :-1, :q], ((0, 0), (0, 0), (1, 0), (0, 0)))
    right = jnp.pad(g[:, :, 1:, q:2 * q], ((0, 0), (0, 0), (0, 1), (0, 0)))
    up = jnp.pad(g[:, :-1, :, 2 * q:3 * q], ((0, 0), (1, 0), (0, 0), (0, 0)))
    down = jnp.pad(g[:, 1:, :, 3 * q:], ((0, 0), (0, 1), (0, 0), (0, 0)))
    return jnp.concatenate([left, right, up, down], axis=-1).reshape(b, n, ch)


def token_shift_context(p):
    half = p.shape[-1] // 2
    return jnp.concatenate([_shift_seq(p[..., :half], 1), _shift_seq(p[..., half:], -1)], axis=-1)


def layer_norm(x, g, b):
    xf = x.astype(jnp.float32)
    mu = xf.mean(-1, keepdims=True)
    var = jnp.square(xf - mu).mean(-1, keepdims=True)
    return ((xf - mu) * lax.rsqrt(var + LN_EPS) * g + b).astype(x.dtype)


def post_norm(x, s, g, b):
    return layer_norm(ALPHA * x + s, g, b)


def modulate(x, shift, scale):
    return x * (1 + scale) + shift


def _heads(t):
    return t.reshape(t.shape[:-1] + (RWKV_HEADS, HEAD_DIM))


def l2_normalize_heads(t):
    th = _heads(t).astype(jnp.float32)
    nrm = jnp.sqrt(jnp.sum(th * th, axis=-1, keepdims=True))
    return (th / jnp.maximum(nrm, 1e-12)).reshape(t.shape).astype(t.dtype)


def group_norm_heads(y, g, b):
    yh = _heads(y).astype(jnp.float32)
    mu = yh.mean(-1, keepdims=True)
    var = jnp.square(yh - mu).mean(-1, keepdims=True)
    out = ((yh - mu) * lax.rsqrt(var + GN_EPS)).reshape(y.shape)
    return (out * g + b).astype(y.dtype)


def rwkv_prepare(p, shift_fn, mu, w0, w2, a0, a2, k_k, k_a):
    pm = p + (shift_fn(p) - p) * mu
    r, k, v, lw_f, lw_b, la_f, la_b, lg = jnp.split(pm, RWKV_SPLITS, axis=-1)
    lw = jnp.stack([lw_f, lw_b])
    la = jnp.stack([la_f, la_b])
    w = jnp.exp(-DECAY_SCALE * jax.nn.sigmoid(w0[:, None, None, :] + jnp.einsum('dblr,drc->dblc', jnp.tanh(lw), w2)))
    a = jax.nn.sigmoid(a0[:, None, None, :] + jnp.einsum('dblr,drc->dblc', la, a2))
    kk = l2_normalize_heads(k * k_k)
    k_dir = k[None] * (1 + (a - 1) * k_a)
    return r, k_dir, v, w, a, kk, lg


def delta_scan(r, w, k, v, kk, a, s0, reverse, emit):
    b, n, ch = r.shape

    def steps(t):
        return t.astype(jnp.float32).reshape(b, n, RWKV_HEADS, HEAD_DIM).transpose(1, 0, 2, 3)

    xs = (steps(r), steps(w), steps(k), steps(v), steps(-kk), steps(kk * a))

    def step(state, inp):
        r_t, w_t, k_t, v_t, ka_t, kb_t = inp
        sa = jnp.einsum('bhvk,bhk->bhv', state, ka_t)
        state = state * w_t[:, :, None, :] + sa[..., None] * kb_t[:, :, None, :] + v_t[..., None] * k_t[:, :, None, :]
        y = jnp.einsum('bhvk,bhk->bhv', state, r_t) if emit else None
        return state, y

    s_fin, ys = lax.scan(step, s0, xs, reverse=reverse)
    if not emit:
        return None, s_fin
    return ys.transpose(1, 0, 2, 3).reshape(b, n, ch).astype(r.dtype), s_fin


def rwkv_finish(r, k_dir, v, lg, y_f, y_b, r_k, lnx_g, lnx_b, g2):
    y = group_norm_heads(y_f + y_b, lnx_g, lnx_b)
    bonus = jnp.sum(_heads(r)[None] * _heads(k_dir) * r_k.reshape(RWKV_HEADS, HEAD_DIM), axis=(0, -1))
    y = y + (bonus[..., None] * _heads(v)).reshape(v.shape)
    return y * (jax.nn.sigmoid(lg) @ g2)


def hyena_filter(n, w1, b1, w2, b2, w3, b3, w_out, freq):
    pos = jnp.arange(n, dtype=jnp.float32)
    t = pos / max(n - 1, 1)
    bands = jnp.linspace(1e-4, FILTER_BANDS - 1, FILTER_BANDS, dtype=jnp.float32)
    ang = (2.0 * math.pi / n) * pos[:, None] * bands[None, :]
    z = jnp.concatenate([t[:, None], jnp.cos(ang), -jnp.sin(ang)], axis=-1)
    h = jnp.sin(freq * (z @ w1 + b1))
    h = jnp.sin(freq * (h @ w2 + b2))
    h = jnp.sin(freq * (h @ w3 + b3))
    filt = (h @ w_out).astype(jnp.float32)
    dist = jnp.abs(pos - n // 2) * (2.0 / n)
    deltas = jnp.abs(jnp.linspace(HYENA_MIN_DECAY, HYENA_MAX_DECAY, HYENA_WIDTH, dtype=jnp.float32))
    filt = filt * jnp.exp(-dist[:, None] * deltas[None, :])
    return filt / jnp.sum(jnp.abs(filt), axis=0, keepdims=True)


def long_conv(z, filt):
    n = z.shape[1]
    zf = jnp.fft.rfft(z.astype(jnp.float32), n=2 * n, axis=1)
    hf = jnp.fft.rfft(filt, n=2 * n, axis=0)
    y = jnp.fft.irfft(zf * hf[None], n=2 * n, axis=1)[:, n // 2:n // 2 + n]
    return y.astype(z.dtype)


def hyena_branch(p, conv_w, conv_b, filt, bias):
    u = _shift_seq(p, 1) * conv_w[0] + p * conv_w[1] + _shift_seq(p, -1) * conv_w[2] + conv_b
    x0, x1, v = jnp.split(u, 3, axis=-1)
    z = v * x1
    z = long_conv(z, filt) + z * bias
    return z * x0


def merge_branches(p_gate, y_rwkv, y_hyena, w_branch, w_out):
    g_r, g_h = jnp.split(jax.nn.sigmoid(p_gate), 2, axis=-1)
    return (g_r * (y_rwkv @ w_branch[0]) + g_h * (y_hyena @ w_branch[1])) @ w_out


def moe_ffn(h, w_router, router_bias, w_gate, w_up, w_down):
    n_tok, d = h.shape
    scores = jax.nn.softmax((h @ w_router).astype(jnp.float32), axis=-1)
    sel = scores + router_bias
    group_score = lax.top_k(sel.reshape(n_tok, N_GROUPS, EXPERTS_PER_GROUP), TOP_K)[0].sum(-1)
    best_group = jnp.argmax(group_score, axis=-1)
    in_group = (jnp.arange(N_EXPERTS) // EXPERTS_PER_GROUP)[None, :] == best_group[:, None]
    _, idx = lax.top_k(jnp.where(in_group, sel, -jnp.inf), TOP_K)
    wts = jnp.take_along_axis(scores, idx, axis=1)
    wts = wts / wts.sum(-1, keepdims=True)

    n_slots = n_tok * TOP_K
    n_blocks = -(-n_slots // MOE_BLOCK) + N_EXPERTS
    flat_e = idx.reshape(-1).astype(jnp.int32)
    flat_tok = jnp.repeat(jnp.arange(n_tok, dtype=jnp.int32), TOP_K)
    order = jnp.argsort(flat_e, stable=True)
    sorted_e = flat_e[order]
    counts = jnp.bincount(flat_e, length=N_EXPERTS)
    padded = ((counts + MOE_BLOCK - 1) // MOE_BLOCK) * MOE_BLOCK
    start = jnp.cumsum(counts) - counts
    pend = jnp.cumsum(padded)
    pstart = pend - padded
    dest = (pstart[sorted_e] + jnp.arange(n_slots) - start[sorted_e]).astype(jnp.int32)
    buf_tok = jnp.full((n_blocks * MOE_BLOCK,), n_tok, jnp.int32).at[dest].set(flat_tok[order])
    block_e = jnp.clip(jnp.searchsorted(pend, jnp.arange(n_blocks) * MOE_BLOCK, side='right'), 0, N_EXPERTS - 1)
    h_pad = jnp.concatenate([h, jnp.zeros((1, d), h.dtype)], axis=0)
    xb = h_pad[buf_tok].reshape(n_blocks, MOE_BLOCK, d)

    def expert_block(args):
        xblk, e = args
        return (jax.nn.silu(xblk @ w_gate[e]) * (xblk @ w_up[e])) @ w_down[e]

    yb = lax.map(expert_block, (xb, block_e)).reshape(n_blocks * MOE_BLOCK, d)
    y_slots = jnp.zeros((n_slots, d), yb.dtype).at[order].set(yb[dest])
    return (y_slots.reshape(n_tok, TOP_K, d) * wts[..., None].astype(yb.dtype)).sum(1)


def setup_inputs(seed: int = 0) -> dict:
    key = jax.random.key(seed)
    ks = iter(jax.random.split(key, 48))
    d = D_MODEL

    def nrm(shape, s):
        return jax.random.normal(next(ks), shape, jnp.float32) * s

    return {
        'x': nrm((BATCH, SEQ, d), 1.0),
        'c': nrm((BATCH, d), 1.0),
        'ctx': nrm((BATCH, CTX_LEN, d), 1.0),
        'c_ctx': nrm((d,), 1.0),
        'w_mod': nrm((DEPTH, d, 6 * d), 0.5 * d ** -0.5),
        'b_mod': nrm((DEPTH, 6 * d), 0.02),
        'w_in': nrm((DEPTH, d, PROJ_COLS), d ** -0.5),
        'rwkv_mu': jax.random.uniform(next(ks), (DEPTH, RWKV_COLS), jnp.float32),
        'rwkv_w0': nrm((DEPTH, 2, RWKV_WIDTH), 0.5),
        'rwkv_w2': nrm((DEPTH, 2, LORA_W, RWKV_WIDTH), 0.5 * LORA_W ** -0.5),
        'rwkv_a0': nrm((DEPTH, 2, RWKV_WIDTH), 0.5),
        'rwkv_a2': nrm((DEPTH, 2, LORA_A, RWKV_WIDTH), 0.5 * LORA_A ** -0.5),
        'rwkv_g2': nrm((DEPTH, LORA_G, RWKV_WIDTH), LORA_G ** -0.5),
        'rwkv_k_k': 0.85 + nrm((DEPTH, RWKV_WIDTH), 0.05),
        'rwkv_k_a': 1.0 + nrm((DEPTH, RWKV_WIDTH), 0.05),
        'rwkv_r_k': nrm((DEPTH, RWKV_WIDTH), 0.1),
        'rwkv_lnx_g': 1.0 + nrm((DEPTH, RWKV_WIDTH), 0.05),
        'rwkv_lnx_b': nrm((DEPTH, RWKV_WIDTH), 0.01),
        'hy_conv_w': nrm((DEPTH, 3, HYENA_COLS), 3 ** -0.5),
        'hy_conv_b': nrm((DEPTH, HYENA_COLS), 0.01),
        'hy_f_w1': nrm((DEPTH, FILTER_EMB, FILTER_HIDDEN), FILTER_EMB ** -0.5),
        'hy_f_b1': nrm((DEPTH, FILTER_HIDDEN), 0.1),
        'hy_f_w2': nrm((DEPTH, FILTER_HIDDEN, FILTER_HIDDEN), FILTER_HIDDEN ** -0.5),
        'hy_f_b2': nrm((DEPTH, FILTER_HIDDEN), 0.1),
        'hy_f_w3': nrm((DEPTH, FILTER_HIDDEN, FILTER_HIDDEN), FILTER_HIDDEN ** -0.5),
        'hy_f_b3': nrm((DEPTH, FILTER_HIDDEN), 0.1),
        'hy_f_wout': nrm((DEPTH, FILTER_HIDDEN, HYENA_WIDTH), FILTER_HIDDEN ** -0.5),
        'hy_freq': 1.0 + nrm((DEPTH, FILTER_HIDDEN), 0.05),
        'hy_bias': nrm((DEPTH, HYENA_WIDTH), 1.0),
        'w_branch': nrm((DEPTH, 2, RWKV_WIDTH, d), RWKV_WIDTH ** -0.5),
        'w_out': nrm((DEPTH, d, d), BETA * d ** -0.5),
        'ln_g': 1.0 + nrm((DEPTH, 2, d), 0.05),
        'ln_b': nrm((DEPTH, 2, d), 0.01),
        'w_router': nrm((d, N_EXPERTS), d ** -0.5),
        'router_bias': nrm((N_EXPERTS,), 0.01),
        'w_gate': nrm((DEPTH, N_EXPERTS, d, D_EXPERT), d ** -0.5),
        'w_up': nrm((DEPTH, N_EXPERTS, d, D_EXPERT), d ** -0.5),
        'w_down': nrm((DEPTH, N_EXPERTS, D_EXPERT, d), BETA * D_EXPERT ** -0.5),
    }


def reference(x, c, ctx, c_ctx, w_mod, b_mod, w_in, rwkv_mu, rwkv_w0, rwkv_w2, rwkv_a0, rwkv_a2, rwkv_g2,
              rwkv_k_k, rwkv_k_a, rwkv_r_k, rwkv_lnx_g, rwkv_lnx_b, hy_conv_w, hy_conv_b, hy_f_w1, hy_f_b1,
              hy_f_w2, hy_f_b2, hy_f_w3, hy_f_b3, hy_f_wout, hy_freq, hy_bias, w_branch, w_out, ln_g, ln_b,
              w_router, router_bias, w_gate, w_up, w_down):
    b, n_lat, d = x.shape
    n_ctx = ctx.shape[1]
    xl, xc = x, ctx
    hy_end = RWKV_COLS + HYENA_COLS
    for l in range(DEPTH):
        last = l == DEPTH - 1
        mod_l = jnp.split((jax.nn.silu(c) @ w_mod[l] + b_mod[l])[:, None, :], 6, axis=-1)
        mod_c = jnp.split(jax.nn.silu(c_ctx) @ w_mod[l] + b_mod[l], 6, axis=-1)
        rw_args = (rwkv_mu[l], rwkv_w0[l], rwkv_w2[l], rwkv_a0[l], rwkv_a2[l], rwkv_k_k[l], rwkv_k_a[l])
        fin_args = (rwkv_r_k[l], rwkv_lnx_g[l], rwkv_lnx_b[l], rwkv_g2[l])
        filt_args = (hy_f_w1[l], hy_f_b1[l], hy_f_w2[l], hy_f_b2[l], hy_f_w3[l], hy_f_b3[l], hy_f_wout[l], hy_freq[l])

        pc = modulate(xc, mod_c[0], mod_c[1]) @ w_in[l]
        pl = modulate(xl, mod_l[0], mod_l[1]) @ w_in[l]
        r_c, kd_c, v_c, w_c, a_c, kk_c, lg_c = rwkv_prepare(pc[..., :RWKV_COLS], token_shift_context, *rw_args)
        r_l, kd_l, v_l, w_l, a_l, kk_l, lg_l = rwkv_prepare(pl[..., :RWKV_COLS], token_shift_latent, *rw_args)
        s0 = jnp.zeros((b, RWKV_HEADS, HEAD_DIM, HEAD_DIM), jnp.float32)
        yc_f, sc_f = delta_scan(r_c, w_c[0], kd_c[0], v_c, kk_c, a_c[0], s0, False, not last)
        yc_b, sc_b = delta_scan(r_c, w_c[1], kd_c[1], v_c, kk_c, a_c[1], s0, True, not last)
        yl_f, _ = delta_scan(r_l, w_l[0], kd_l[0], v_l, kk_l, a_l[0], sc_f, False, True)
        yl_b, _ = delta_scan(r_l, w_l[1], kd_l[1], v_l, kk_l, a_l[1], sc_b, True, True)
        ro_l = rwkv_finish(r_l, kd_l, v_l, lg_l, yl_f, yl_b, *fin_args)
        ho_l = hyena_branch(pl[..., RWKV_COLS:hy_end], hy_conv_w[l], hy_conv_b[l],
                            hyena_filter(n_lat, *filt_args), hy_bias[l])
        mix_l = merge_branches(pl[..., hy_end:], ro_l, ho_l, w_branch[l], w_out[l])
        xl = post_norm(xl, mod_l[2] * mix_l, ln_g[l, 0], ln_b[l, 0])

        if last:
            hf = modulate(xl, mod_l[3], mod_l[4]).reshape(b * n_lat, d)
            yf = moe_ffn(hf, w_router, router_bias, w_gate[l], w_up[l], w_down[l]).reshape(b, n_lat, d)
            xl = post_norm(xl, mod_l[5] * yf, ln_g[l, 1], ln_b[l, 1])
        else:
            ro_c = rwkv_finish(r_c, kd_c, v_c, lg_c, yc_f, yc_b, *fin_args)
            ho_c = hyena_branch(pc[..., RWKV_COLS:hy_end], hy_conv_w[l], hy_conv_b[l],
                                hyena_filter(n_ctx, *filt_args), hy_bias[l])
            mix_c = merge_branches(pc[..., hy_end:], ro_c, ho_c, w_branch[l], w_out[l])
            xc = post_norm(xc, mod_c[2] * mix_c, ln_g[l, 0], ln_b[l, 0])
            hf = jnp.concatenate([modulate(xc, mod_c[3], mod_c[4]), modulate(xl, mod_l[3], mod_l[4])], axis=1)
            yf = moe_ffn(hf.reshape(b * (n_ctx + n_lat), d), w_router, router_bias,
                         w_gate[l], w_up[l], w_down[l]).reshape(b, n_ctx + n_lat, d)
            xc = post_norm(xc, mod_c[5] * yf[:, :n_ctx], ln_g[l, 1], ln_b[l, 1])
            xl = post_norm(xl, mod_l[5] * yf[:, n_ctx:], ln_g[l, 1], ln_b[l, 1])
    return xl
```

```python
import contextlib
import math
import numpy as np
import concourse.bass as bass
import concourse.mybir as mybir
from concourse.ap import AP
from concourse.bass_utils import run_bass_kernel_spmd


F32 = mybir.dt.float32
BF16 = mybir.dt.bfloat16
ALU = mybir.AluOpType
AF = mybir.ActivationFunctionType
AX = mybir.AxisListType


F32 = mybir.dt.float32
BF16 = mybir.dt.bfloat16
ALU = mybir.AluOpType
AF = mybir.ActivationFunctionType
AX = mybir.AxisListType


class Buf:
    __slots__ = ("t", "lw", "rd", "name")

    def __init__(self, t, name=""):
        self.t = t
        self.lw = None
        self.rd = {}
        self.name = name

    def __getitem__(self, k):
        return self.t[k]


class Sched:
    NDSEM = 16

    LIMIT = 3000

    def __init__(self, nc, stack):
        self.nc = nc
        self.stack = stack
        self.eng = {"pe": nc.tensor, "act": nc.scalar, "dve": nc.vector, "pool": nc.gpsimd, "sp": nc.sync}
        self.sems = {}
        self.cnt = {}
        self.epoch = {}
        self.nsem = 0
        self.waited = {e: {} for e in self.eng}
        self.pending = {e: [] for e in self.eng}
        for e in self.eng:
            self.epoch[e] = 0
            self.sems[(e, 0)] = self._newsem()
            self.cnt[e] = 0
        self.dsem_uses = {}
        self.dkey = {}
        self.dnext = {"hw": 0, "sw": 0}
        self.dn = {"hw": self.NDSEM, "sw": 8}
        for kind in ("hw", "sw"):
            for i in range(self.dn[kind]):
                key = ("d", kind, i, 0)
                self.sems[key] = self._newsem()
                self.dkey[(kind, i)] = key
                self.dsem_uses[(kind, i)] = 0
        self.nbuf = 0

    def _newsem(self):
        self.nsem += 1
        return self.stack.enter_context(self.nc.semaphore("sm%d" % self.nsem))

    def sb(self, shape, dt=F32, name=None):
        name = ("s_" + name) if name else None
        self.nbuf += 1
        name = name or "sb%d" % self.nbuf
        t = self.stack.enter_context(self.nc.sbuf_tensor(name, list(shape), dt))
        return Buf(t, name)

    def ps(self, shape, dt=F32, name=None):
        name = ("p_" + name) if name else None
        self.nbuf += 1
        name = name or "ps%d" % self.nbuf
        t = self.stack.enter_context(self.nc.psum_tensor(name, list(shape), dt))
        return Buf(t, name)

    def dram(self, ap, name=""):
        return Buf(ap, name)

    def _wait(self, e, deps):
        w = self.waited[e]
        eng = self.eng[e]
        for (s, v) in deps:
            if w.get(s, 0) < v:
                eng.wait_ge(self.sems[s], v)
                w[s] = v

    def _deps(self, reads, writes):
        deps = {}

        def add(ev):
            if ev is None:
                return
            s, v = ev
            if deps.get(s, 0) < v:
                deps[s] = v
        for b in reads:
            add(b.lw)
        for b in writes:
            add(b.lw)
            for s, v in b.rd.items():
                add((s, v))
        return list(deps.items())

    def _commit(self, ev, reads, writes):
        for b in writes:
            b.lw = ev
            b.rd = {}
        for b in reads:
            if b.rd.get(ev[0], 0) < ev[1]:
                b.rd[ev[0]] = ev[1]

    def op(self, e, fn, reads=(), writes=(), inc=True):
        self._wait(e, self._deps(reads, writes))
        inst = fn(self.eng[e])
        if not inc:
            self.pending[e].append((list(reads), list(writes)))
            return None
        if self.cnt[e] >= self.LIMIT:
            self.epoch[e] += 1
            self.cnt[e] = 0
            self.sems[(e, self.epoch[e])] = self._newsem()
        self.cnt[e] += 1
        key = (e, self.epoch[e])
        inst.then_inc(self.sems[key], 1)
        ev = (key, self.cnt[e])
        for (r, w) in self.pending[e]:
            self._commit(ev, r, w)
        self.pending[e] = []
        self._commit(ev, reads, writes)
        return ev

    def dma(self, q, out, in_, reads=(), writes=(), relay=False, **kw):
        kw_relay = relay
        kind = "sw" if q == "pool" else "hw"
        k = (kind, self.dnext[kind])
        self.dnext[kind] = (self.dnext[kind] + 1) % self.dn[kind]
        key = self.dkey[k]
        deps = self._deps(reads, writes)
        if self.dsem_uses[k] > 0:
            deps.append((key, 16 * self.dsem_uses[k]))
        self._wait(q, deps)
        if 16 * (self.dsem_uses[k] + 1) > self.LIMIT:
            key = ("d", kind, k[1], key[3] + 1)
            self.sems[key] = self._newsem()
            self.dkey[k] = key
            self.dsem_uses[k] = 0
        self.dsem_uses[k] += 1
        self.eng[q].dma_start(out=out, in_=in_, **kw).then_inc(self.sems[key], 16)
        ev = (key, 16 * self.dsem_uses[k])
        if kw_relay:
            self._wait(q, [ev])
            if self.cnt[q] >= self.LIMIT:
                self.epoch[q] += 1
                self.cnt[q] = 0
                self.sems[(q, self.epoch[q])] = self._newsem()
            self.cnt[q] += 1
            k2 = (q, self.epoch[q])
            self.eng[q].nop().then_inc(self.sems[k2], 1)
            ev = (k2, self.cnt[q])
        self._commit(ev, reads, writes)
        return ev

    def finish(self, bufs):
        deps = self._deps(bufs, [])
        self._wait("sp", deps)


D = 1024
PROJ = 5504
NG = 4
GW = 640
NOWN = 2048
NCTX = 256
TO = NOWN + NCTX
OUT_NAMES = ["r", "v", "nkk", "lwf", "lwb", "kdf", "kdb", "kbf", "kbb", "bv", "gmul", "z", "x0"]
OUT_BASE = {n: 4 * i for i, n in enumerate(OUT_NAMES)}
OUT_BASE["gates"] = 4 * len(OUT_NAMES)
OUT_CH = OUT_BASE["gates"] + 16
PV = {}
_c = 0
for _n, _k in [("mu", 15), ("ml", 15), ("mr", 15), ("mup", 15), ("mdn", 15), ("mprev", 15), ("mnext", 15),
               ("w0", 8), ("a0", 8), ("k_k", 4), ("k_a", 4), ("r_k", 4), ("cw0", 12), ("cw1", 12), ("cw2", 12),
               ("cb", 12), ("bmod", 48), ("flags", 2)]:
    PV[_n] = (_c, _k)
    _c += _k
NPV = _c
DECAY_SCALE = 0.606531


def build_ka():
    nc = bass.Bass("TRN2", target_bir_lowering=False)
    xh = nc.dram_tensor("xh", [NG * 512 + 128 + NCTX, D], F32, kind="ExternalInput").ap()
    cc = nc.dram_tensor("cc", [128, 16], F32, kind="ExternalInput").ap()
    wmod = nc.dram_tensor("wmod", [D, 2048], F32, kind="ExternalInput").ap()
    win = nc.dram_tensor("win", [D, PROJ], F32, kind="ExternalInput").ap()
    pvec = nc.dram_tensor("pvec", [128, NPV], F32, kind="ExternalInput").ap()
    w2s = nc.dram_tensor("w2s", [128, 512], F32, kind="ExternalInput").ap()
    a2s = nc.dram_tensor("a2s", [128, 512], F32, kind="ExternalInput").ap()
    g2 = nc.dram_tensor("g2", [128, 512], F32, kind="ExternalInput").ap()
    cst = nc.dram_tensor("cst", [128, 256], F32, kind="ExternalInput").ap()
    out = nc.dram_tensor("out", [OUT_CH * 128, TO], F32, kind="ExternalOutput").ap()
    with contextlib.ExitStack() as st:
        S = Sched(nc, st)
        d_in = S.dram(xh)
        d_out = S.dram(out)
        d_w = S.dram(win)
        pv = S.sb([128, NPV], F32, "pv")
        S.dma("sp", pv[:], pvec, reads=[d_in], writes=[pv])
        cs_ = S.sb([128, 256], F32, "cstsb")
        S.dma("sp", cs_[:], cst, reads=[d_in], writes=[cs_])
        ident = cs_[:, 0:128]
        bones = cs_[:, 128:256]
        w2b = S.sb([128, 512], F32, "w2b")
        a2b = S.sb([128, 512], F32, "a2b")
        g2b = S.sb([128, 512], F32, "g2b")
        S.dma("sp", w2b[:], w2s, reads=[d_in], writes=[w2b])
        S.dma("sp", a2b[:], a2s, reads=[d_in], writes=[a2b])
        S.dma("sp", g2b[:], g2, reads=[d_in], writes=[g2b])

        def pvc(name, i=0, n=1):
            b, k = PV[name]
            return pv[:, b + i:b + i + n]

        der = S.sb([128, 64], F32, "der")
        S.op("dve", lambda e: e.tensor_scalar(out=der[:, 0:15], in0=pvc("mu", 0, 15), scalar1=-1.0, scalar2=1.0,
                                               op0=ALU.mult, op1=ALU.add), reads=[pv], writes=[der])
        S.op("dve", lambda e: e.tensor_scalar(out=der[:, 15:19], in0=pvc("k_a", 0, 4), scalar1=-1.0, scalar2=1.0,
                                               op0=ALU.mult, op1=ALU.add), reads=[pv], writes=[der])
        mdir = S.sb([128, 6 * 15], F32, "mdir")
        for i, nm in enumerate(["ml", "mr", "mup", "mdn", "mprev", "mnext"]):
            S.op("dve", lambda e, i=i, nm=nm: e.tensor_tensor(out=mdir[:, 15 * i:15 * i + 15], in0=pvc("mu", 0, 15),
                                                             in1=pvc(nm, 0, 15), op=ALU.mult), reads=[pv], writes=[mdir])
        sc = S.sb([128, 16], F32, "sc")
        S.dma("sp", sc[:], cc, reads=[d_in], writes=[sc])
        scs = S.sb([128, 16], F32, "scs")
        S.op("act", lambda e: e.activation(out=scs[:], in_=sc[:], func=AF.Silu), reads=[sc], writes=[scs])
        scr = S.sb([128, 8, 2], F32, "scr")
        S.op("dve", lambda e: e.tensor_copy(out=scr[:, :, 0], in_=scs[:, 0:8]), reads=[scs], writes=[scr])
        S.op("dve", lambda e: e.tensor_copy(out=scr[:, :, 1], in_=scs[:, 8:16]), reads=[scs], writes=[scr])
        modv = S.sb([128, 16, 2], F32, "modv")
        wm = [S.sb([128, 8, 128], F32, "wm%d" % i) for i in range(2)]
        pmod = S.ps([128, 2, 2], F32, "pmod")
        wmv = wmod.rearrange("(k p) n -> p k n", p=128)
        bm0 = PV["bmod"][0]
        for q in range(16):
            wb_ = wm[q % 2]
            S.dma("sp", wb_[:], wmv[:, :, q * 128:(q + 1) * 128], reads=[d_in], writes=[wb_])
            for k in range(8):
                S.op("pe", lambda e, k=k, wb_=wb_: e.matmul(pmod[:, q % 2, :], wb_[:, k, :], scr[:, k, :],
                                                         start=(k == 0), stop=(k == 7)), reads=[wb_, scr], writes=[pmod], inc=(k == 7))
            S.op("dve", lambda e, q=q: e.tensor_scalar(out=modv[:, q, :], in0=pmod[:, q % 2, :], scalar1=pv[:, bm0 + q:bm0 + q + 1],
                                                       scalar2=None, op0=ALU.add), reads=[pmod, pv], writes=[modv])
        S.op("dve", lambda e: e.tensor_scalar_add(out=modv[:, 8:16, :], in0=modv[:, 8:16, :], scalar1=1.0),
             reads=[modv], writes=[modv])
        wb = [S.sb([128, PROJ], BF16, "wb%d" % k) for k in range(8)]
        winv = win.rearrange("(k p) n -> p k n", p=128)
        for k in range(8):
            S.dma("pool", wb[k][:], winv[:, k, :], reads=[d_w], writes=[wb[k]])

        xt = [S.sb([128, D], F32, "xt%d" % i) for i in range(2)]
        xmT = S.sb([128, 8, GW], BF16, "xmT")
        ptr = [S.ps([128, 4, 128], F32, "ptr%d" % i) for i in range(2)]
        pmm = [S.ps([128, 512], F32, "pmm%d" % i) for i in range(3)]
        PJ = [S.sb([128, GW], F32, "pj%d" % i) for i in range(2)]
        PM = [S.sb([128, 512], F32, "pm%d" % i) for i in range(15)]
        TMP = [S.sb([128, 512], F32, "tmp%d" % i) for i in range(7)]
        OST = [S.sb([128, 512], F32, "ost%d" % i) for i in range(4)]
        AD = [[S.sb([128, 512], F32, "ad%d_%d" % (d, c)) for c in range(4)] for d in range(2)]
        outs = []
        state = {"pj": 0, "pmm": 0, "ost": 0, "tmp": 0, "xt": 0, "ptr": 0, "q": 0}

        def nxt(key, lst):
            i = state[key]
            state[key] = (i + 1) % len(lst)
            return lst[i]

        def emit(name, ci, src_buf, src_ap, col0, n):
            row = (OUT_BASE[name] + ci) * 128
            q = "sp" if state["q"] % 2 == 0 else "act"
            state["q"] += 1
            ob_ = S.dram(out)
            outs.append(ob_)
            S.dma(q, out[row:row + 128, col0:col0 + n], src_ap, reads=[src_buf], writes=[ob_])

        for g in range(NG + 1):
            ctx = g == NG
            ntile = 2 if ctx else 5
            W = 256 if ctx else GW
            N = 256 if ctx else 512
            o0 = 0 if ctx else 64
            row0 = NG * 512 + 128 if ctx else g * 512
            col0 = NOWN if ctx else g * 512
            sidx = 1 if ctx else 0
            for t in range(ntile):
                x_ = nxt("xt", xt)
                S.dma("sp", x_[:], xh[row0 + t * 128:row0 + (t + 1) * 128, :], reads=[d_in], writes=[x_])
                for hh in range(2):
                    p_ = nxt("ptr", ptr)
                    for kk in range(4):
                        k = hh * 4 + kk
                        S.op("pe", lambda e, p_=p_, kk=kk, k=k, x_=x_: e.transpose(out=p_[:, kk, :], in_=x_[:, k * 128:(k + 1) * 128],
                                                                                  identity=ident), reads=[x_, cs_], writes=[p_], inc=(kk == 3))
                    for kk in range(4):
                        k = hh * 4 + kk
                        S.op("act", lambda e, p_=p_, kk=kk, k=k, t=t: e.activation(
                            out=xmT[:, k, t * 128:(t + 1) * 128], in_=p_[:, kk, :], func=AF.Identity,
                            scale=modv[:, 8 + k, sidx:sidx + 1], bias=modv[:, k, sidx:sidx + 1]),
                            reads=[p_, modv], writes=[xmT])
            if g == 0:
                fb = PV["flags"][0]
                S.op("dve", lambda e: e.tensor_scalar_mul(out=xmT[:, :, 0:64], in0=xmT[:, :, 0:64], scalar1=pv[:, fb:fb + 1]),
                     reads=[xmT, pv], writes=[xmT])
            if g == NG - 1:
                fb = PV["flags"][0] + 1
                S.op("dve", lambda e: e.tensor_scalar_mul(out=xmT[:, :, 576:640], in0=xmT[:, :, 576:640], scalar1=pv[:, fb:fb + 1]),
                     reads=[xmT, pv], writes=[xmT])

            def proj(j, c0, n):
                p_ = nxt("pmm", pmm)
                for k in range(8):
                    S.op("pe", lambda e, k=k, p_=p_: e.matmul(p_[:, 0:n], wb[k][:, j * 128:(j + 1) * 128], xmT[:, k, c0:c0 + n],
                                                             start=(k == 0), stop=(k == 7)), reads=[wb[k], xmT], writes=[p_], inc=(k == 7))
                return p_

            def proj_full(j):
                pj = nxt("pj", PJ)
                if ctx:
                    p_ = proj(j, 0, 256)
                    S.op("act", lambda e: e.copy(out=pj[:, 0:256], in_=p_[:, 0:256]), reads=[p_], writes=[pj])
                else:
                    p_ = proj(j, 0, 512)
                    S.op("act", lambda e: e.copy(out=pj[:, 0:512], in_=p_[:, 0:512]), reads=[p_], writes=[pj])
                    p2 = proj(j, 512, 128)
                    S.op("act", lambda e: e.copy(out=pj[:, 512:640], in_=p2[:, 0:128]), reads=[p2], writes=[pj])
                return pj

            for j in range(15):
                pj = proj_full(j)
                pm = PM[j]
                S.op("dve", lambda e: e.tensor_scalar_mul(out=pm[:, 0:N], in0=pj[:, o0:o0 + N], scalar1=der[:, j:j + 1]),
                     reads=[pj, der], writes=[pm])

                def addsh(dst, src, mi):
                    S.op("dve", lambda e: e.scalar_tensor_tensor(out=dst, in0=src, scalar=mdir[:, 15 * mi + j:15 * mi + j + 1],
                                                                 in1=dst, op0=ALU.mult, op1=ALU.add),
                         reads=[pj, mdir, pm], writes=[pm])
                if ctx:
                    if j <= 7:
                        addsh(pm[:, 1:256], pj[:, 0:255], 4)
                    if j >= 7:
                        addsh(pm[:, 0:255], pj[:, 1:256], 5)
                else:
                    pmv = pm[:, 0:512].rearrange("p (r c) -> p r c", c=64)
                    pjv = pj[:, 64:576].rearrange("p (r c) -> p r c", c=64)
                    if j <= 3:
                        addsh(pmv[:, :, 1:64], pjv[:, :, 0:63], 0)
                    if 3 <= j <= 7:
                        addsh(pmv[:, :, 0:63], pjv[:, :, 1:64], 1)
                    if 7 <= j <= 11:
                        addsh(pm[:, 0:512], pj[:, 0:512], 2)
                    if j >= 11:
                        addsh(pm[:, 0:512], pj[:, 128:640], 3)
            R = PM[0:4]; K = PM[4:8]; V = PM[8:12]; LW = PM[12]; LA = PM[13]; LG = PM[14]
            for c in range(4):
                emit("r", c, R[c], R[c][:, 0:N], col0, N)
                emit("v", c, V[c], V[c][:, 0:N], col0, N)
            th = nxt("tmp", TMP)
            S.op("act", lambda e: e.activation(out=th[:, 0:N], in_=LW[:, 0:N], func=AF.Tanh), reads=[LW], writes=[th])
            w0b = PV["w0"][0]; a0b = PV["a0"][0]
            for d in range(2):
                for c in range(4):
                    p_ = nxt("pmm", pmm)
                    S.op("pe", lambda e, p_=p_: e.matmul(p_[:, 0:N], w2b[64 * d:64 * d + 64, c * 128:(c + 1) * 128],
                                                        th[64 * d:64 * d + 64, 0:N], start=True, stop=True),
                         reads=[w2b, th], writes=[p_])
                    o_ = nxt("ost", OST)
                    S.op("act", lambda e, p_=p_, o_=o_: e.activation(out=o_[:, 0:N], in_=p_[:, 0:N], func=AF.Sigmoid,
                                                                     bias=pv[:, w0b + 4 * d + c:w0b + 4 * d + c + 1], scale=1.0),
                         reads=[p_, pv], writes=[o_])
                    S.op("dve", lambda e, o_=o_: e.tensor_scalar_mul(out=o_[:, 0:N], in0=o_[:, 0:N], scalar1=-DECAY_SCALE),
                         reads=[o_], writes=[o_])
                    emit("lwf" if d == 0 else "lwb", c, o_, o_[:, 0:N], col0, N)
                    p2 = nxt("pmm", pmm)
                    S.op("pe", lambda e, p2=p2: e.matmul(p2[:, 0:N], a2b[64 * d:64 * d + 64, c * 128:(c + 1) * 128],
                                                        LA[64 * d:64 * d + 64, 0:N], start=True, stop=True),
                         reads=[a2b, LA], writes=[p2])
                    a_ = AD[d][c]
                    S.op("act", lambda e, p2=p2, a_=a_: e.activation(out=a_[:, 0:N], in_=p2[:, 0:N], func=AF.Sigmoid,
                                                                     bias=pv[:, a0b + 4 * d + c:a0b + 4 * d + c + 1], scale=1.0),
                         reads=[p2, pv], writes=[a_])
            kkb = PV["k_k"][0]; kab = PV["k_a"][0]; rkb = PV["r_k"][0]
            for c in range(4):
                kx = nxt("tmp", TMP)
                S.op("dve", lambda e: e.tensor_scalar_mul(out=kx[:, 0:N], in0=K[c][:, 0:N], scalar1=pv[:, kkb + c:kkb + c + 1]),
                     reads=[K[c], pv], writes=[kx])
                sq = nxt("tmp", TMP)
                S.op("act", lambda e: e.activation(out=sq[:, 0:N], in_=kx[:, 0:N], func=AF.Square), reads=[kx], writes=[sq])
                p_ = nxt("pmm", pmm)
                S.op("pe", lambda e: e.matmul(p_[:, 0:N], bones, sq[:, 0:N], start=True, stop=True), reads=[cs_, sq], writes=[p_])
                rn = nxt("tmp", TMP)
                S.op("dve", lambda e: e.tensor_scalar(out=rn[:, 0:N], in0=p_[:, 0:N], scalar1=1e-24, scalar2=None,
                                                       op0=ALU.max), reads=[p_], writes=[rn])
                S.op("act", lambda e: e.activation(out=rn[:, 0:N], in_=rn[:, 0:N], func=AF.Sqrt), reads=[rn], writes=[rn])
                S.op("dve", lambda e: e.reciprocal(out=rn[:, 0:N], in_=rn[:, 0:N]), reads=[rn], writes=[rn])
                kk_ = nxt("tmp", TMP)
                S.op("dve", lambda e: e.tensor_tensor(out=kk_[:, 0:N], in0=kx[:, 0:N], in1=rn[:, 0:N], op=ALU.mult),
                     reads=[kx, rn], writes=[kk_])
                o_ = nxt("ost", OST)
                S.op("dve", lambda e: e.tensor_scalar_mul(out=o_[:, 0:N], in0=kk_[:, 0:N], scalar1=-1.0), reads=[kk_], writes=[o_])
                emit("nkk", c, o_, o_[:, 0:N], col0, N)
                ksum = nxt("tmp", TMP)
                for d in range(2):
                    a_ = AD[d][c]
                    ob = nxt("ost", OST)
                    S.op("dve", lambda e, ob=ob, a_=a_: e.tensor_tensor(out=ob[:, 0:N], in0=kk_[:, 0:N], in1=a_[:, 0:N], op=ALU.mult),
                         reads=[kk_, a_], writes=[ob])
                    emit("kbf" if d == 0 else "kbb", c, ob, ob[:, 0:N], col0, N)
                    t2 = nxt("tmp", TMP)
                    S.op("dve", lambda e, t2=t2, a_=a_: e.tensor_scalar(out=t2[:, 0:N], in0=a_[:, 0:N], scalar1=pv[:, kab + c:kab + c + 1],
                                                                        scalar2=der[:, 15 + c:16 + c], op0=ALU.mult, op1=ALU.add),
                         reads=[a_, pv, der], writes=[t2])
                    od = nxt("ost", OST)
                    S.op("dve", lambda e, od=od, t2=t2: e.tensor_tensor(out=od[:, 0:N], in0=t2[:, 0:N], in1=K[c][:, 0:N], op=ALU.mult),
                         reads=[t2, K[c]], writes=[od])
                    emit("kdf" if d == 0 else "kdb", c, od, od[:, 0:N], col0, N)
                    if d == 0:
                        S.op("dve", lambda e, od=od: e.tensor_copy(out=ksum[:, 0:N], in_=od[:, 0:N]), reads=[od], writes=[ksum])
                    else:
                        S.op("dve", lambda e, od=od: e.tensor_tensor(out=ksum[:, 0:N], in0=ksum[:, 0:N], in1=od[:, 0:N], op=ALU.add),
                             reads=[od, ksum], writes=[ksum])
                S.op("dve", lambda e: e.scalar_tensor_tensor(out=ksum[:, 0:N], in0=R[c][:, 0:N], scalar=pv[:, rkb + c:rkb + c + 1],
                                                             in1=ksum[:, 0:N], op0=ALU.mult, op1=ALU.mult),
                     reads=[R[c], pv, ksum], writes=[ksum])
                p3 = nxt("pmm", pmm)
                S.op("pe", lambda e: e.matmul(p3[:, 0:N], bones, ksum[:, 0:N], start=True, stop=True), reads=[cs_, ksum], writes=[p3])
                ob2 = nxt("ost", OST)
                S.op("dve", lambda e: e.tensor_tensor(out=ob2[:, 0:N], in0=p3[:, 0:N], in1=V[c][:, 0:N], op=ALU.mult),
                     reads=[p3, V[c]], writes=[ob2])
                emit("bv", c, ob2, ob2[:, 0:N], col0, N)
            sg = nxt("tmp", TMP)
            S.op("act", lambda e: e.activation(out=sg[:, 0:N], in_=LG[:, 0:N], func=AF.Sigmoid), reads=[LG], writes=[sg])
            for c in range(4):
                p_ = nxt("pmm", pmm)
                S.op("pe", lambda e, p_=p_: e.matmul(p_[:, 0:N], g2b[:, c * 128:(c + 1) * 128], sg[:, 0:N], start=True, stop=True),
                     reads=[g2b, sg], writes=[p_])
                o_ = nxt("ost", OST)
                S.op("act", lambda e, p_=p_, o_=o_: e.copy(out=o_[:, 0:N], in_=p_[:, 0:N]), reads=[p_], writes=[o_])
                emit("gmul", c, o_, o_[:, 0:N], col0, N)
            c0b = PV["cw0"][0]; c1b = PV["cw1"][0]; c2b = PV["cw2"][0]; cbb = PV["cb"][0]
            for jj in range(12):
                j = 15 + jj
                pj = proj_full(j)
                u = PM[jj]
                S.op("dve", lambda e: e.tensor_scalar(out=u[:, 0:N], in0=pj[:, o0:o0 + N], scalar1=pv[:, c1b + jj:c1b + jj + 1],
                                                       scalar2=pv[:, cbb + jj:cbb + jj + 1], op0=ALU.mult, op1=ALU.add),
                     reads=[pj, pv], writes=[u])
                if ctx:
                    S.op("dve", lambda e: e.scalar_tensor_tensor(out=u[:, 1:256], in0=pj[:, 0:255], scalar=pv[:, c0b + jj:c0b + jj + 1],
                                                                 in1=u[:, 1:256], op0=ALU.mult, op1=ALU.add), reads=[pj, pv, u], writes=[u])
                    S.op("dve", lambda e: e.scalar_tensor_tensor(out=u[:, 0:255], in0=pj[:, 1:256], scalar=pv[:, c2b + jj:c2b + jj + 1],
                                                                 in1=u[:, 0:255], op0=ALU.mult, op1=ALU.add), reads=[pj, pv, u], writes=[u])
                else:
                    S.op("dve", lambda e: e.scalar_tensor_tensor(out=u[:, 0:512], in0=pj[:, 63:575], scalar=pv[:, c0b + jj:c0b + jj + 1],
                                                                 in1=u[:, 0:512], op0=ALU.mult, op1=ALU.add), reads=[pj, pv, u], writes=[u])
                    S.op("dve", lambda e: e.scalar_tensor_tensor(out=u[:, 0:512], in0=pj[:, 65:577], scalar=pv[:, c2b + jj:c2b + jj + 1],
                                                                 in1=u[:, 0:512], op0=ALU.mult, op1=ALU.add), reads=[pj, pv, u], writes=[u])
            for c in range(4):
                emit("x0", c, PM[c], PM[c][:, 0:N], col0, N)
                o_ = nxt("ost", OST)
                S.op("dve", lambda e, o_=o_: e.tensor_tensor(out=o_[:, 0:N], in0=PM[8 + c][:, 0:N], in1=PM[4 + c][:, 0:N], op=ALU.mult),
                     reads=[PM[8 + c], PM[4 + c]], writes=[o_])
                emit("z", c, o_, o_[:, 0:N], col0, N)
            for jj in range(16):
                p_ = proj(27 + jj, o0, N)
                o_ = nxt("ost", OST)
                S.op("act", lambda e, p_=p_, o_=o_: e.activation(out=o_[:, 0:N], in_=p_[:, 0:N], func=AF.Sigmoid), reads=[p_], writes=[o_])
                emit("gates", jj, o_, o_[:, 0:N], col0, N)
        S.finish(outs)
    return nc


def host_pvec(inp, l, core):
    pv = np.zeros((128, NPV), np.float32)

    def put(name, vec):
        b, k = PV[name]
        pv[:, b:b + k] = np.asarray(vec, np.float32).reshape(k, 128).T
    put("mu", inp["rwkv_mu"][l])
    col = np.arange(1920)
    qd = col // 480
    put("ml", (qd == 0)); put("mr", (qd == 1)); put("mup", (qd == 2)); put("mdn", (qd == 3))
    put("mprev", (col < 960)); put("mnext", (col >= 960))
    put("w0", inp["rwkv_w0"][l].reshape(-1)); put("a0", inp["rwkv_a0"][l].reshape(-1))
    put("k_k", inp["rwkv_k_k"][l]); put("k_a", inp["rwkv_k_a"][l]); put("r_k", inp["rwkv_r_k"][l])
    cw = inp["hy_conv_w"][l]
    put("cw0", cw[0]); put("cw1", cw[1]); put("cw2", cw[2]); put("cb", inp["hy_conv_b"][l])
    put("bmod", inp["b_mod"][l])
    b, _ = PV["flags"]
    pv[:, b] = 0.0 if core == 0 else 1.0
    pv[:, b + 1] = 0.0 if core == 7 else 1.0
    return pv


def host_cst():
    c = np.zeros((128, 256), np.float32)
    c[:, 0:128] = np.eye(128)
    c[0:64, 128:192] = 1.0
    c[64:128, 192:256] = 1.0
    return c


def ka_inputs(inp, l, xl, xc, core):
    lo = core * 2048 - 64
    xh = np.zeros((NG * 512 + 128 + NCTX, D), np.float32)
    a, b = max(lo, 0), min(lo + 2176, 16384)
    xh[a - lo:b - lo] = xl[a:b]
    xh[2176:] = xc
    cc = np.concatenate([inp["c"][0].reshape(8, 128).T, inp["c_ctx"].reshape(8, 128).T], axis=1)
    return dict(xh=xh, cc=np.ascontiguousarray(cc, np.float32), wmod=np.ascontiguousarray(inp["w_mod"][l][:, 0:2048]),
                win=inp["w_in"][l], pvec=host_pvec(inp, l, core),
                w2s=np.ascontiguousarray(inp["rwkv_w2"][l].reshape(128, 512)),
                a2s=np.ascontiguousarray(inp["rwkv_a2"][l].reshape(128, 512)),
                g2=inp["rwkv_g2"][l], cst=host_cst())


TT = 16640
BLK = 640
NBUF = 2
NDIR = 1
NBLK = TT // BLK
NBUFY = NBLK
NPAIR = BLK // 128


def build_kb1(nblk=NBLK):
    nc = bass.Bass("TRN2", target_bir_lowering=False)
    sin = [nc.dram_tensor("sin%d" % d, [6 * 64, TT], F32, kind="ExternalInput").ap() for d in range(NDIR)]
    cmask = nc.dram_tensor("cmask", [128, 6 * 128], F32, kind="ExternalInput").ap()
    rmask = nc.dram_tensor("rmask", [64, BLK], F32, kind="ExternalInput").ap()
    yout = [nc.dram_tensor("y%d" % d, [64, TT], F32, kind="ExternalOutput").ap() for d in range(NDIR)]
    with contextlib.ExitStack() as st:
        S = Sched(nc, st)
        d_in = S.dram(sin[0])
        outs = []
        cm = S.sb([128, 6 * 128], F32, "cm")
        S.dma("sp", cm[:], cmask, reads=[d_in], writes=[cm])
        rm = S.sb([64, BLK], F32, "rm")
        S.dma("sp", rm[:], rmask, reads=[d_in], writes=[rm])
        identf = cm[:, 5 * 128:6 * 128]
        idb = S.sb([128, 128], BF16, "idb")
        S.op("dve", lambda e: e.tensor_copy(out=idb[:], in_=identf), reads=[cm], writes=[idb])

        XB = [[S.sb([64, 6, BLK], F32, "xb%d_%d" % (d, i)) for i in range(NBUF)] for d in range(NDIR)]
        CS = S.sb([64, BLK], F32, "cs"); EIN = [S.sb([64, BLK], F32, "ein%d" % d) for d in range(NDIR)]
        EEX = S.sb([64, BLK], F32, "eex"); ENG = S.sb([64, BLK], F32, "eng")
        AT = S.sb([64, BLK], BF16, "at"); BT = S.sb([64, BLK], BF16, "bt"); KT = S.sb([64, BLK], BF16, "kt")
        RTB = S.sb([64, BLK], BF16, "rtb"); VB = S.sb([64, BLK], BF16, "vb"); RTF = S.sb([64, BLK], F32, "rtf")
        YB = [[S.sb([64, BLK], F32, "yb%d_%d" % (d, i)) for i in range(NBUFY)] for d in range(NDIR)]
        ST = [[S.sb([64, 64], F32, "st%d_%d" % (d, i)) for i in range(2)] for d in range(NDIR)]
        for d in range(NDIR):
            S.op("dve", lambda e, d=d: e.memset(ST[d][0][:], 0.0), writes=[ST[d][0]])
        pb0 = S.ps([128, 512], F32, "pb0")
        pb1 = S.ps([128, 512], F32, "pb1")
        pb2 = S.ps([128, 512], F32, "pb2")
        ptp = S.ps([128, 4, 64], BF16, "ptp")
        pb4 = S.ps([128, 512], F32, "pb4")
        pb5 = S.ps([128, 512], F32, "pb5")
        P_A = Buf(pb0.t[:, :]); P_RK = Buf(pb1.t[:, 0:128]); P_U = Buf(pb1.t[:, 128:256]); P_WV = Buf(pb1.t[:, 256:384])
        P_X = Buf(pb1.t[:, 384:448]); P_Q = Buf(pb2.t[:, 0:256]); P_QE = Buf(pb2.t[0:64, 256:384]); P_YI = Buf(pb2.t[0:64, 384:512])
        P_MG = Buf(pb4.t[0:64, 0:256]); P_CY = Buf(pb5.t[0:64, 0:64]); P_CS = Buf(pb5.t[0:64, 64:128])
        TTt = S.sb([128, 4, 64], BF16, "tt")
        AM = S.sb([128, 5, 128], BF16, "am")
        PP = [S.sb([128, 128], BF16, "pp%d" % i) for i in range(2)]
        MM = [S.sb([128, 2, 128], BF16, "mm%d" % i) for i in range(2)]
        AXt = S.sb([128, 128], BF16, "ax")
        WV = S.sb([128, 128], BF16, "wv")
        QE = [S.sb([64, 128], F32, "qe%d" % i) for i in range(2)]
        YI = [S.sb([64, 128], F32, "yi%d" % i) for i in range(2)]
        MC = [S.sb([64, 2, 64], F32, "mc%d" % i) for i in range(2)]
        GS = [S.sb([64, 2, 64], F32, "gs%d" % i) for i in range(2)]
        pc = [0]

        for b in range(nblk):
            for d in range(NDIR):
                X = XB[d][b % NBUF]
                S.dma("sp" if d == 0 else "act", X[:], sin[d][:, b * BLK:(b + 1) * BLK].rearrange("(q p) t -> p q t", p=64),
                      reads=[d_in], writes=[X])
                R_, V_, NKK, LW, KD, KB = (X[:, i, :] for i in range(6))
                ein = EIN[d]
                S.op("dve", lambda e: e.tensor_tensor_scan(CS[:], rm[:], LW, 0.0, ALU.mult, ALU.add), reads=[rm, X], writes=[CS])
                S.op("act", lambda e: e.activation(out=ein[:], in_=CS[:], func=AF.Exp), reads=[CS], writes=[ein])
                S.op("act", lambda e: e.activation(out=ENG[:], in_=CS[:], func=AF.Exp, scale=-1.0), reads=[CS], writes=[ENG])
                S.op("dve", lambda e: e.tensor_tensor(out=EEX[:], in0=CS[:], in1=LW, op=ALU.subtract), reads=[CS, X], writes=[EEX])
                S.op("act", lambda e: e.activation(out=EEX[:], in_=EEX[:], func=AF.Exp), reads=[EEX], writes=[EEX])
                S.op("dve", lambda e: e.tensor_tensor(out=AT[:], in0=NKK, in1=EEX[:], op=ALU.mult), reads=[X, EEX], writes=[AT])
                S.op("dve", lambda e: e.tensor_tensor(out=BT[:], in0=KB, in1=ENG[:], op=ALU.mult), reads=[X, ENG], writes=[BT])
                S.op("dve", lambda e: e.tensor_tensor(out=KT[:], in0=KD, in1=ENG[:], op=ALU.mult), reads=[X, ENG], writes=[KT])
                S.op("dve", lambda e: e.tensor_tensor(out=RTF[:], in0=R_, in1=ein[:], op=ALU.mult), reads=[X, ein], writes=[RTF])
                S.op("act", lambda e: e.copy(out=RTB[:], in_=RTF[:]), reads=[RTF], writes=[RTB])
                S.op("act", lambda e: e.copy(out=VB[:], in_=V_), reads=[X], writes=[VB])
                Yb = YB[d][b]
                for p in range(NPAIR):
                    a = p * 128
                    sl = slice(a, a + 128)
                    k2 = pc[0] % 2
                    pc[0] += 1
                    qe, yi, mc, gs = QE[k2], YI[k2], MC[k2], GS[k2]
                    for i, src in enumerate([AT, BT, KT, VB]):
                        S.op("pe", lambda e, i=i, src=src: e.transpose(out=ptp[:, i, :], in_=src[:, sl], identity=idb[0:64, 0:64]),
                             reads=[src, idb], writes=[ptp])
                    S.op("act", lambda e: e.copy(out=TTt[:], in_=ptp[:]), reads=[ptp], writes=[TTt])
                    S.op("act", lambda e: e.copy(out=AXt[:, 0:64], in_=ptp[:, 0, :]), reads=[ptp], writes=[AXt])
                    for i, (l_, r_) in enumerate([(BT, AT), (AT, BT), (KT, AT), (BT, RTB)]):
                        S.op("pe", lambda e, i=i, l_=l_, r_=r_: e.matmul(P_A[:, i * 128:(i + 1) * 128], l_[:, sl], r_[:, sl], start=True, stop=True),
                             reads=[l_, r_], writes=[P_A])
                    S.op("pe", lambda e: e.matmul(P_RK[:, :], KT[:, sl], RTB[:, sl], start=True, stop=True), reads=[KT, RTB], writes=[P_RK])
                    S.op("dve", lambda e: e.tensor_tensor(out=AM[:, 0:4, :], in0=P_A[:, :].rearrange("p (q t) -> p q t", q=4),
                                                          in1=cm[:, 0:512].rearrange("p (q t) -> p q t", q=4), op=ALU.mult),
                         reads=[P_A, cm], writes=[AM])
                    S.op("dve", lambda e: e.tensor_tensor(out=AM[:, 4, :], in0=P_RK[:, :], in1=cm[:, 512:640], op=ALU.mult),
                         reads=[P_RK, cm], writes=[AM])
                    Pc = PP[0]
                    S.op("dve", lambda e: e.tensor_tensor(out=Pc[:], in0=AM[:, 0, :], in1=identf, op=ALU.add), reads=[AM, cm], writes=[Pc])
                    Mcur_M = AM[:, 0, :]; Mcur_T = AM[:, 1, :]; Mbuf = AM
                    for lvl in range(5):
                        Mn = MM[lvl % 2]
                        if lvl < 4:
                            S.op("pe", lambda e: e.matmul(P_Q[:, 0:128], Mcur_T, Mcur_M, start=True, stop=True), reads=[Mbuf], writes=[P_Q])
                        S.op("pe", lambda e: e.matmul(P_Q[:, 128:256], Mcur_M, Mcur_T, start=True, stop=True), reads=[Mbuf], writes=[P_Q])
                        if lvl < 4:
                            S.op("act", lambda e: e.copy(out=Mn[:, :, :], in_=P_Q[:, :].rearrange("p (q t) -> p q t", q=2)), reads=[P_Q], writes=[Mn])
                        else:
                            S.op("act", lambda e: e.copy(out=Mn[:, 1, :], in_=P_Q[:, 128:256]), reads=[P_Q], writes=[Mn])
                        S.op("pe", lambda e: e.matmul(P_U[:, :], Mn[:, 1, :], Pc[:], start=True, stop=True), reads=[Mn, Pc], writes=[P_U])
                        Pn = PP[(lvl + 1) % 2]
                        S.op("dve", lambda e: e.tensor_tensor(out=Pn[:], in0=P_U[:, :], in1=Pc[:], op=ALU.add), reads=[P_U, Pc], writes=[Pn])
                        Pc = Pn
                        Mcur_M = Mn[:, 0, :]; Mcur_T = Mn[:, 1, :]; Mbuf = Mn
                    Tm = Pc
                    S.op("pe", lambda e: e.matmul(P_X[:, :], AM[:, 2, :], TTt[:, 3, :], start=True, stop=True), reads=[AM, TTt], writes=[P_X])
                    S.op("act", lambda e: e.copy(out=AXt[:, 64:128], in_=P_X[:, :]), reads=[P_X], writes=[AXt])
                    S.op("pe", lambda e: e.matmul(P_WV[:, :], Tm[:], AXt[:], start=True, stop=True), reads=[Tm, AXt], writes=[P_WV])
                    S.op("act", lambda e: e.copy(out=WV[:], in_=P_WV[:, :]), reads=[P_WV], writes=[WV])
                    S.op("pe", lambda e: e.matmul(P_QE[:, :], WV[:, 0:64], AM[:, 3, :], start=True, stop=True), reads=[WV, AM], writes=[P_QE])
                    S.op("dve", lambda e: e.tensor_tensor(out=qe[:], in0=P_QE[:, :], in1=RTF[:, sl], op=ALU.add), reads=[P_QE, RTF], writes=[qe])
                    S.op("pe", lambda e: e.matmul(P_YI[:, :], WV[:, 64:128], AM[:, 3, :], start=True, stop=False), reads=[WV, AM], writes=[P_YI])
                    S.op("pe", lambda e: e.matmul(P_YI[:, :], TTt[:, 3, :], AM[:, 4, :], start=False, stop=True), reads=[TTt, AM], writes=[P_YI])
                    S.op("act", lambda e: e.copy(out=yi[:], in_=P_YI[:, :]), reads=[P_YI], writes=[yi])
                    for c in range(2):
                        rs = slice(64 * c, 64 * c + 64)
                        S.op("pe", lambda e: e.matmul(P_MG[:, c * 64:(c + 1) * 64], WV[rs, 0:64], TTt[rs, 1, :], start=True, stop=True),
                             reads=[WV, TTt], writes=[P_MG])
                        S.op("pe", lambda e: e.matmul(P_MG[:, 128 + c * 64:128 + (c + 1) * 64], TTt[rs, 1, :], WV[rs, 64:128], start=True, stop=False),
                             reads=[WV, TTt], writes=[P_MG])
                        S.op("pe", lambda e: e.matmul(P_MG[:, 128 + c * 64:128 + (c + 1) * 64], TTt[rs, 2, :], TTt[rs, 3, :], start=False, stop=True),
                             reads=[TTt], writes=[P_MG])
                    for c in range(2):
                        S.op("dve", lambda e: e.tensor_tensor(out=mc[:, c, :], in0=P_MG[:, c * 64:(c + 1) * 64], in1=identf[0:64, 0:64], op=ALU.add),
                             reads=[P_MG, cm], writes=[mc])
                        col = a + 64 * c + 63
                        S.op("dve", lambda e: e.tensor_scalar_mul(out=gs[:, c, :], in0=P_MG[:, 128 + c * 64:128 + (c + 1) * 64],
                                                                  scalar1=ein[:, col:col + 1]), reads=[P_MG, ein], writes=[gs])
                    for c in range(2):
                        gi = (b * NPAIR + p) * 2 + c
                        s_cur = ST[d][gi % 2]; s_nxt = ST[d][(gi + 1) % 2]
                        S.op("pe", lambda e: e.matmul(P_CY[:, :], s_cur[:], qe[:, c * 64:(c + 1) * 64], start=True, stop=True),
                             reads=[s_cur, qe], writes=[P_CY])
                        S.op("dve", lambda e: e.tensor_tensor(out=Yb[:, a + c * 64:a + (c + 1) * 64], in0=P_CY[:, :], in1=yi[:, c * 64:(c + 1) * 64],
                                                              op=ALU.add), reads=[P_CY, yi], writes=[Yb])
                        S.op("pe", lambda e: e.matmul(P_CS[:, :], mc[:, c, :], s_cur[:], start=True, stop=True), reads=[mc, s_cur], writes=[P_CS])
                        col = a + 64 * c + 63
                        S.op("dve", lambda e: e.scalar_tensor_tensor(out=s_nxt[:], in0=P_CS[:, :], scalar=ein[:, col:col + 1], in1=gs[:, c, :],
                                                                     op0=ALU.mult, op1=ALU.add), reads=[P_CS, ein, gs], writes=[s_nxt])
                ob = S.dram(yout[d]); outs.append(ob)
                S.dma("act", yout[d][:, b * BLK:(b + 1) * BLK], Yb[:], reads=[Yb], writes=[ob])
        S.finish(outs)
    return nc


def kb1_consts():
    idx = np.arange(128)
    same = (idx[:, None] // 64) == (idx[None, :] // 64)
    ms = (same & (idx[:, None] < idx[None, :])).astype(np.float32)
    msT = (same & (idx[:, None] > idx[None, :])).astype(np.float32)
    mi = (same & (idx[:, None] <= idx[None, :])).astype(np.float32)
    cmask = np.concatenate([ms, msT, ms, mi, mi, np.eye(128, dtype=np.float32)], axis=1)
    rmask = np.ones((64, BLK), np.float32)
    rmask[:, ::64] = 0.0
    return dict(cmask=np.ascontiguousarray(cmask), rmask=rmask)


I32 = mybir.dt.int32
NL = 16384
NC_ = 256
FB = 1024
TWO_PI = 2.0 * math.pi


def build_kb2(nch=64):
    nc = bass.Bass("TRN2", target_bir_lowering=False)
    z = nc.dram_tensor("z", [64, NL], F32, kind="ExternalInput").ap()
    zc = nc.dram_tensor("zc", [64, NC_], F32, kind="ExternalInput").ap()
    fwm = nc.dram_tensor("fwm", [64, 256], F32, kind="ExternalInput").ap()
    fvec = nc.dram_tensor("fvec", [64, 10], F32, kind="ExternalInput").ap()
    cst = nc.dram_tensor("cst", [128, 256], F32, kind="ExternalInput").ap()
    gpl = nc.dram_tensor("gpl", [64, NL + 256], BF16, kind="Internal").ap()
    gpc = nc.dram_tensor("gpc", [64, NC_ + 256], BF16, kind="Internal").ap()
    yl = nc.dram_tensor("yl", [64, NL], F32, kind="ExternalOutput").ap()
    yc = nc.dram_tensor("yc", [64, NC_], F32, kind="ExternalOutput").ap()
    with contextlib.ExitStack() as st:
        S = Sched(nc, st)
        d_in = S.dram(z)
        outs = []
        fw = S.sb([64, 256], F32, "fw"); fv = S.sb([64, 10], F32, "fv"); cs_ = S.sb([128, 256], F32, "cstb")
        S.dma("sp", fw[:], fwm, reads=[d_in], writes=[fw])
        S.dma("sp", fv[:], fvec, reads=[d_in], writes=[fv])
        S.dma("sp", cs_[:], cst, reads=[d_in], writes=[cs_])
        ident = cs_[:, 0:128]; antiI = cs_[:, 128:256]
        FILT = S.sb([64, NL], F32, "filt")
        SUMS = S.sb([64, 16], F32, "sums")
        POS = S.sb([64, FB], F32, "pos"); VAL = S.sb([64, FB], F32, "val"); KI = S.sb([64, FB], I32, "ki")
        KF = S.sb([64, FB], F32, "kf"); RR = S.sb([64, FB], F32, "rr")
        HH = [S.sb([64, FB], F32, "hh%d" % i) for i in range(2)]
        DEC = S.sb([64, FB], F32, "dec"); ABS_ = S.sb([64, FB], F32, "abs")
        GB = S.sb([64, FB], BF16, "gb"); ZT = S.sb([64, 128], BF16, "zt")
        pf = [S.ps([64, 512], F32, "pf%d" % i) for i in range(2)]
        S.op("dve", lambda e: e.memset(ZT[:], 0.0), writes=[ZT])
        pfi = [0]

        def rr_sin(dst, src, rows, w):
            S.op("dve", lambda e: e.tensor_scalar(out=KI[0:rows, 0:w], in0=src[0:rows, 0:w], scalar1=1.0 / TWO_PI, scalar2=None, op0=ALU.mult),
                 reads=[src], writes=[KI])
            S.op("dve", lambda e: e.tensor_copy(out=KF[0:rows, 0:w], in_=KI[0:rows, 0:w]), reads=[KI], writes=[KF])
            S.op("dve", lambda e: e.scalar_tensor_tensor(out=RR[0:rows, 0:w], in0=KF[0:rows, 0:w], scalar=-TWO_PI, in1=src[0:rows, 0:w],
                                                         op0=ALU.mult, op1=ALU.add), reads=[KF, src], writes=[RR])
            S.op("dve", lambda e: e.tensor_scalar(out=RR[0:rows, 0:w], in0=RR[0:rows, 0:w], scalar1=-3.141592, scalar2=3.141592,
                                                   op0=ALU.max, op1=ALU.min), reads=[RR], writes=[RR])
            S.op("act", lambda e: e.activation(out=dst[0:rows, 0:w], in_=RR[0:rows, 0:w], func=AF.Sin), reads=[RR], writes=[dst])

        def gen_filter(n, gp, esc_col, dsc_col):
            gpb = S.dram(gp)
            nblk = max(1, n // FB)
            w = min(n, FB)
            for blk in range(nblk):
                j0 = blk * w
                S.op("pool", lambda e: e.iota(POS[:, 0:w], pattern=[[-1, w]], base=n - 1 - j0, channel_multiplier=0,
                                              allow_small_or_imprecise_dtypes=True), writes=[POS])
                S.op("dve", lambda e: e.tensor_scalar(out=VAL[0:33, 0:w], in0=POS[0:33, 0:w], scalar1=fv[0:33, esc_col:esc_col + 1],
                                                       scalar2=fv[0:33, 6:7], op0=ALU.mult, op1=ALU.add), reads=[POS, fv], writes=[VAL])
                h = HH[0]
                rr_sin(h, VAL, 33, w)
                S.op("dve", lambda e: e.tensor_copy(out=h[0:1, 0:w], in_=VAL[0:1, 0:w]), reads=[VAL, h], writes=[h])
                K = 33
                for li in range(3):
                    for pc_ in range((w + 511) // 512):
                        cw = min(512, w - pc_ * 512)
                        p_ = pf[pfi[0] % 2]; pfi[0] += 1
                        S.op("pe", lambda e: e.matmul(p_[:, 0:cw], fw[0:K, li * 64:(li + 1) * 64], h[0:K, pc_ * 512:pc_ * 512 + cw],
                                                      start=True, stop=True), reads=[fw, h], writes=[p_])
                        S.op("dve", lambda e: e.tensor_scalar(out=VAL[:, pc_ * 512:pc_ * 512 + cw], in0=p_[:, 0:cw], scalar1=fv[:, li:li + 1],
                                                               scalar2=fv[:, 3:4], op0=ALU.add, op1=ALU.mult), reads=[p_, fv], writes=[VAL])
                    hn = HH[(li + 1) % 2]
                    rr_sin(hn, VAL, 64, w)
                    h = hn
                    K = 64
                S.op("dve", lambda e: e.tensor_scalar(out=DEC[:, 0:w], in0=POS[:, 0:w], scalar1=-float(n // 2), scalar2=None,
                                                       op0=ALU.add), reads=[POS], writes=[DEC])
                S.op("act", lambda e: e.activation(out=DEC[:, 0:w], in_=DEC[:, 0:w], func=AF.Abs), reads=[DEC], writes=[DEC])
                S.op("act", lambda e: e.activation(out=DEC[:, 0:w], in_=DEC[:, 0:w], func=AF.Exp, scale=fv[:, dsc_col:dsc_col + 1]),
                     reads=[DEC, fv], writes=[DEC])
                for pc_ in range((w + 511) // 512):
                    cw = min(512, w - pc_ * 512)
                    p_ = pf[pfi[0] % 2]; pfi[0] += 1
                    S.op("pe", lambda e: e.matmul(p_[:, 0:cw], fw[:, 192:256], h[:, pc_ * 512:pc_ * 512 + cw], start=True, stop=True),
                         reads=[fw, h], writes=[p_])
                    S.op("dve", lambda e: e.tensor_tensor(out=FILT[:, j0 + pc_ * 512:j0 + pc_ * 512 + cw], in0=p_[:, 0:cw],
                                                          in1=DEC[:, pc_ * 512:pc_ * 512 + cw], op=ALU.mult), reads=[p_, DEC], writes=[FILT])
                S.op("act", lambda e: e.activation(out=ABS_[:, 0:w], in_=FILT[:, j0:j0 + w], func=AF.Abs), reads=[FILT], writes=[ABS_])
                S.op("dve", lambda e: e.reduce_sum(out=SUMS[:, blk:blk + 1], in_=ABS_[:, 0:w], axis=AX.X), reads=[ABS_], writes=[SUMS])
            tot = S.sb([64, 1], F32, "tot%d" % n)
            S.op("dve", lambda e: e.reduce_sum(out=tot[:], in_=SUMS[:, 0:nblk], axis=AX.X), reads=[SUMS], writes=[tot])
            S.op("dve", lambda e: e.reciprocal(out=tot[:], in_=tot[:]), reads=[tot], writes=[tot])
            S.dma("sp", gp[:, 0:128], ZT[:], reads=[ZT], writes=[gpb])
            S.dma("sp", gp[:, 128 + n:256 + n], ZT[:], reads=[ZT], writes=[gpb])
            for blk in range(nblk):
                j0 = blk * w
                S.op("dve", lambda e: e.tensor_scalar_mul(out=GB[:, 0:w], in0=FILT[:, j0:j0 + w], scalar1=tot[:, 0:1]), reads=[FILT, tot], writes=[GB])
                S.dma("sp", gp[:, 128 + j0:128 + j0 + w], GB[:, 0:w], reads=[GB], writes=[gpb])
            return gpb

        gpb_c = gen_filter(NC_, gpc, 7, 8)
        gpb_l = gen_filter(NL, gpl, 5, 4)

        G = [S.sb([128, NL + 128], BF16, "g%d" % i) for i in range(2)]
        GQ = [[Buf(G[i].t[:, q * 4128:(q + 1) * 4128]) for q in range(4)] for i in range(2)]
        ZN = [S.sb([128, 128], F32, "zn%d" % i) for i in range(2)]
        ZP = [S.sb([128, 256], BF16, "zp%d" % i) for i in range(2)]
        OS = [S.sb([128, 128], F32, "os%d" % i) for i in range(2)]
        YO = [S.sb([128, 128], F32, "yo%d" % i) for i in range(2)]
        ptz = [S.ps([128, 128], F32, "ptz%d" % i) for i in range(2)]
        pcv = [S.ps([128, 128], F32, "pcv%d" % i) for i in range(2)]
        pun = [S.ps([128, 128], F32, "pun%d" % i) for i in range(2)]
        for i in range(2):
            S.op("dve", lambda e, i=i: e.memset(ZP[i][:], 0.0), writes=[ZP[i]])
        it = [0]

        def conv(n, zsrc, gp, gpb, ydst):
            nb = n // 128
            H = n // 2
            padc = nb // 2
            for c in range(nch):
                k = it[0] % 2; it[0] += 1
                g_, zn, zp, os_, yo = G[k], ZN[k], ZP[k], OS[k], YO[k]
                nq = 4 if n > 1024 else 1
                qw = (n + 128) // nq
                gq = GQ[k][0:nq]
                for qi in range(nq):
                    src = AP(gp.tensor, c * (n + 256) + qi * qw, [[1, 128], [1, qw]])
                    S.dma("sp" if (qi + k) % 2 == 0 else "act", g_[:, qi * qw:(qi + 1) * qw], src, reads=[gpb], writes=[gq[qi]])
                S.dma("pool", zn[0:nb, :], zsrc[c, :].rearrange("(a b) -> a b", b=128), reads=[d_in], writes=[zn])
                S.op("pe", lambda e: e.transpose(out=ptz[k][:, 0:nb], in_=zn[0:nb, :], identity=ident[0:nb, 0:nb]), reads=[zn, cs_], writes=[ptz[k]])
                S.op("act", lambda e: e.copy(out=zp[:, padc:padc + nb], in_=ptz[k][:, 0:nb]), reads=[ptz[k]], writes=[zp])
                ds = list(range(-(nb // 2), nb // 2 + 1))
                for i, d in enumerate(ds):
                    S.op("pe", lambda e: e.matmul(pcv[k][:, 0:nb], g_[:, H - 128 * d:H - 128 * d + 128], zp[:, padc - d:padc - d + nb],
                                                  start=(i == 0), stop=(i == len(ds) - 1)), reads=gq + [zp], writes=[pcv[k]], inc=(i == len(ds) - 1))
                S.op("act", lambda e: e.copy(out=os_[:, 0:nb], in_=pcv[k][:, 0:nb]), reads=[pcv[k]], writes=[os_])
                S.op("pe", lambda e: e.matmul(pun[k][0:nb, :], os_[:, 0:nb], antiI, start=True, stop=True), reads=[os_, cs_], writes=[pun[k]])
                S.op("dve", lambda e: e.tensor_copy(out=yo[0:nb, :], in_=pun[k][0:nb, :]), reads=[pun[k]], writes=[yo])
                ob = S.dram(ydst); outs.append(ob)
                S.dma("pool", ydst[c, :].rearrange("(a b) -> a b", b=128), yo[0:nb, :], reads=[yo], writes=[ob])

        conv(NC_, zc, gpc, gpb_c, yc)
        for i in range(2):
            S.op("dve", lambda e, i=i: e.memset(ZP[i][:], 0.0), writes=[ZP[i]])
        conv(NL, z, gpl, gpb_l, yl)
        S.finish(outs)
    return nc


def kb2_inputs(inp, l, core, z_l, z_c):
    cs = slice(64 * core, 64 * core + 64)
    fwm = np.zeros((64, 256), np.float32)
    fwm[0:33, 0:64] = inp["hy_f_w1"][l]
    fwm[:, 64:128] = inp["hy_f_w2"][l]
    fwm[:, 128:192] = inp["hy_f_w3"][l]
    fwm[:, 192:256] = inp["hy_f_wout"][l][:, cs]
    fv = np.zeros((64, 10), np.float32)
    fv[:, 0] = inp["hy_f_b1"][l]; fv[:, 1] = inp["hy_f_b2"][l]; fv[:, 2] = inp["hy_f_b3"][l]; fv[:, 3] = inp["hy_freq"][l]
    min_d = math.log(1e-2) / 1.5; max_d = math.log(1e-2) / 0.3
    deltas = np.abs(np.linspace(min_d, max_d, 512, dtype=np.float32))[cs]
    bands = np.linspace(1e-4, 15, 16, dtype=np.float32)
    for n, ecol, dcol in [(NL, 5, 4), (NC_, 7, 8)]:
        fv[:, dcol] = -deltas * (2.0 / n)
        fv[0, ecol] = 1.0 / (n - 1)
        fv[1:17, ecol] = (2.0 * math.pi / n) * bands
        fv[17:33, ecol] = (2.0 * math.pi / n) * bands
    fv[1:17, 6] = math.pi / 2
    fv[17:33, 6] = math.pi
    cst = np.zeros((128, 256), np.float32)
    cst[:, 0:128] = np.eye(128); cst[:, 128:256] = np.eye(128)[::-1]
    return dict(z=np.ascontiguousarray(z_l[cs], np.float32), zc=np.ascontiguousarray(z_c[cs], np.float32), fwm=fwm, fvec=fv, cst=cst)


D = 1024
TO = 2304
FA = {"yf": 0, "yb": 4, "bv": 8, "gmul": 12, "z": 16, "x0": 20, "zc": 24, "gates": 28}
FA_CH = 44
PV2 = {"lnxg": 0, "lnxb": 4, "hb": 8, "bmod": 12}
NPV2 = 12 + 48
ALPHA = 8 ** 0.25
GN_EPS = 6.4e-4
LN_EPS = 1e-5
BIG = 1e30


def build_kc1():
    nc = bass.Bass("TRN2", target_bir_lowering=False)
    fa = nc.dram_tensor("fa", [FA_CH * 128, TO], F32, kind="ExternalInput").ap()
    xres = nc.dram_tensor("xres", [TO, D], F32, kind="ExternalInput").ap()
    cc = nc.dram_tensor("cc", [128, 16], F32, kind="ExternalInput").ap()
    wmod = nc.dram_tensor("wmod", [D, 3072], F32, kind="ExternalInput").ap()
    pvec = nc.dram_tensor("pvec", [128, NPV2], F32, kind="ExternalInput").ap()
    rowp = nc.dram_tensor("rowp", [4, D], F32, kind="ExternalInput").ap()
    wbr = nc.dram_tensor("wbr", [D, D], F32, kind="ExternalInput").ap()
    wout = nc.dram_tensor("wout", [D, D], F32, kind="ExternalInput").ap()
    wrt = nc.dram_tensor("wrt", [D, 16], F32, kind="ExternalInput").ap()
    cst = nc.dram_tensor("cst", [128, 512], F32, kind="ExternalInput").ap()
    xl1 = nc.dram_tensor("xl1", [TO, D], F32, kind="ExternalOutput").ap()
    hT = nc.dram_tensor("hT", [D, TO], F32, kind="ExternalOutput").ap()
    wts = nc.dram_tensor("wts", [TO, 16], F32, kind="ExternalOutput").ap()
    with contextlib.ExitStack() as st:
        S = Sched(nc, st)
        d_in = S.dram(fa)
        outs = []
        pv = S.sb([128, NPV2], F32, "pv"); S.dma("sp", pv[:], pvec, reads=[d_in], writes=[pv])
        cs_ = S.sb([128, 512], F32, "cstb"); S.dma("sp", cs_[:], cst, reads=[d_in], writes=[cs_])
        ident = cs_[:, 0:128]; bones = cs_[:, 128:256]
        LNG = S.sb([128, D], F32, "lng"); LNB = S.sb([128, D], F32, "lnb"); BM2 = S.sb([128, D], F32, "bm2")
        RB = S.sb([128, 16], F32, "rb")
        for i, t_ in enumerate([LNG, LNB, BM2]):
            S.dma("sp", t_[:], AP(rowp.tensor, i * D, [[0, 128], [1, D]]), reads=[d_in], writes=[t_])
        S.dma("sp", RB[:], AP(rowp.tensor, 3 * D, [[0, 128], [1, 16]]), reads=[d_in], writes=[RB])
        wr = S.sb([128, 8, 16], F32, "wr")
        S.dma("sp", wr[:], wrt.rearrange("(k p) n -> p k n", p=128), reads=[d_in], writes=[wr])
        wbr_b = S.sb([128, 4, D], BF16, "wbr_b"); wbh_b = S.sb([128, 4, D], BF16, "wbh_b"); wo_b = S.sb([128, 8, D], BF16, "wo_b")
        S.dma("pool", wbr_b[:], wbr[0:512, :].rearrange("(k p) n -> p k n", p=128), reads=[d_in], writes=[wbr_b])
        S.dma("pool", wbh_b[:], wbr[512:1024, :].rearrange("(k p) n -> p k n", p=128), reads=[d_in], writes=[wbh_b])
        S.dma("pool", wo_b[:], wout.rearrange("(k p) n -> p k n", p=128), reads=[d_in], writes=[wo_b])
        sc = S.sb([128, 16], F32, "sc"); S.dma("sp", sc[:], cc, reads=[d_in], writes=[sc])
        scs = S.sb([128, 16], F32, "scs")
        S.op("act", lambda e: e.activation(out=scs[:], in_=sc[:], func=AF.Silu), reads=[sc], writes=[scs])
        scr = S.sb([128, 8, 2], F32, "scr")
        S.op("dve", lambda e: e.tensor_copy(out=scr[:, :, 0], in_=scs[:, 0:8]), reads=[scs], writes=[scr])
        S.op("dve", lambda e: e.tensor_copy(out=scr[:, :, 1], in_=scs[:, 8:16]), reads=[scs], writes=[scr])
        pmm = [S.ps([128, 512], F32, "pmm%d" % i) for i in range(4)]
        ptr = [S.ps([128, 4, 128], F32, "ptr%d" % i) for i in range(2)]
        psm = S.ps([128, 512], F32, "psm")
        prow = S.ps([128, 512], F32, "prow")
        P_MOD = Buf(psm.t[:, 0:4]); P_RT = Buf(psm.t[:, 16:32])
        state = {"pmm": 0, "ptr": 0, "ld": 0, "tmp": 0, "q": 0}

        def nxt(key, lst):
            i = state[key]; state[key] = (i + 1) % len(lst); return lst[i]
        wmv = wmod.rearrange("(k p) n -> p k n", p=128)
        wm5 = [S.sb([128, 8, 512], F32, "wm5_0")] * 2
        g2row = S.sb([2, D], F32, "g2row")
        G2B = [S.sb([128, D], F32, "g2b%d" % s) for s in range(2)]
        for hf in range(2):
            S.dma("sp", wm5[hf][:], wmv[:, :, hf * 512:(hf + 1) * 512], reads=[d_in], writes=[wm5[hf]])
            for k in range(8):
                S.op("pe", lambda e, k=k: e.matmul(prow[0:2, :], scr[:, k, :], wm5[hf][:, k, :], start=(k == 0), stop=(k == 7)),
                     reads=[scr, wm5[hf]], writes=[prow], inc=(k == 7))
            S.op("act", lambda e: e.copy(out=g2row[:, hf * 512:(hf + 1) * 512], in_=prow[0:2, :]), reads=[prow], writes=[g2row])
        for s in range(2):
            for hf in range(2):
                p_ = nxt("pmm", pmm)
                S.op("pe", lambda e: e.matmul(p_[:, :], cs_[0:2, 256 + 128 * s:256 + 128 * s + 128], g2row[:, hf * 512:(hf + 1) * 512],
                                              start=True, stop=True), reads=[cs_, g2row], writes=[p_])
                S.op("dve", lambda e: e.tensor_tensor(out=G2B[s][:, hf * 512:(hf + 1) * 512], in0=p_[:, :], in1=BM2[:, hf * 512:(hf + 1) * 512],
                                                      op=ALU.add), reads=[p_, BM2], writes=[G2B[s]])
        modv = S.sb([128, 16, 2], F32, "modv")
        wm = [S.sb([128, 8, 128], F32, "wm%d" % i) for i in range(2)]
        bm0 = PV2["bmod"]
        for q in range(16):
            wb_ = wm[q % 2]
            S.dma("sp", wb_[:], wmv[:, :, 1024 + q * 128:1024 + (q + 1) * 128], reads=[d_in], writes=[wb_])
            for k in range(8):
                S.op("pe", lambda e, k=k: e.matmul(P_MOD[:, 2 * (q % 2):2 * (q % 2) + 2], wb_[:, k, :], scr[:, k, :], start=(k == 0), stop=(k == 7)),
                     reads=[wb_, scr], writes=[P_MOD], inc=(k == 7))
            S.op("dve", lambda e: e.tensor_scalar(out=modv[:, q, :], in0=P_MOD[:, 2 * (q % 2):2 * (q % 2) + 2], scalar1=pv[:, bm0 + 24 + q:bm0 + 25 + q],
                                                   scalar2=None, op0=ALU.add), reads=[P_MOD, pv], writes=[modv])
        S.op("dve", lambda e: e.tensor_scalar_add(out=modv[:, 8:16, :], in0=modv[:, 8:16, :], scalar1=1.0), reads=[modv], writes=[modv])

        LD = [S.sb([128, 512], F32, "ld%d" % i) for i in range(8)]
        TMP = [S.sb([128, 512], F32, "tmp%d" % i) for i in range(6)]
        roT = S.sb([128, 4, 512], BF16, "roT"); hoT = S.sb([128, 4, 512], BF16, "hoT"); mT = S.sb([128, 8, 512], BF16, "mT")
        XT = [S.sb([128, D], F32, "xt%d" % i) for i in range(2)]
        T1 = [S.sb([128, D], F32, "t1_%d" % i) for i in range(2)]
        SQ = S.sb([128, D], F32, "sq")
        XO = [S.sb([128, D], F32, "xo%d" % i) for i in range(2)]
        HT = [S.sb([128, 8, 128], F32, "ht%d" % i) for i in range(2)]
        SM = [S.sb([128, 8], F32, "sm%d" % i) for i in range(2)]
        RT = [S.sb([128, 12, 16], F32, "rt%d" % i) for i in range(2)]
        PSr = [S.sb([128, 4, 6], F32, "psr%d" % i) for i in range(2)]
        GS4 = [S.sb([128, 8], F32, "gs4_%d" % i) for i in range(2)]
        tcount = [0]

        def load(ch, col0, n):
            t_ = nxt("ld", LD)
            q = "sp" if state["q"] % 2 == 0 else "act"
            state["q"] += 1
            S.dma(q, t_[:, 0:n], fa[ch * 128:(ch + 1) * 128, col0:col0 + n], reads=[d_in], writes=[t_])
            return t_

        def outdma(dst_ap, src_buf, src_ap, q="sp"):
            ob = S.dram(xl1); outs.append(ob)
            S.dma(q, dst_ap, src_ap, reads=[src_buf], writes=[ob])

        for g in range(5):
            ctx = g == 4
            N = 256 if ctx else 512
            col0 = 2048 if ctx else g * 512
            s = 1 if ctx else 0
            for c in range(4):
                yf = load(FA["yf"] + c, col0, N); yb = load(FA["yb"] + c, col0, N)
                bv = load(FA["bv"] + c, col0, N); gm = load(FA["gmul"] + c, col0, N)
                y = nxt("tmp", TMP)
                S.op("dve", lambda e: e.tensor_tensor(out=y[:, 0:N], in0=yf[:, 0:N], in1=yb[:, 0:N], op=ALU.add), reads=[yf, yb], writes=[y])
                p_ = nxt("pmm", pmm)
                S.op("pe", lambda e: e.matmul(p_[:, 0:N], bones, y[:, 0:N], start=True, stop=True), reads=[cs_, y], writes=[p_])
                yc = nxt("tmp", TMP)
                S.op("dve", lambda e: e.scalar_tensor_tensor(out=yc[:, 0:N], in0=p_[:, 0:N], scalar=-1.0 / 64, in1=y[:, 0:N], op0=ALU.mult, op1=ALU.add),
                     reads=[p_, y], writes=[yc])
                sq = nxt("tmp", TMP)
                S.op("act", lambda e: e.activation(out=sq[:, 0:N], in_=yc[:, 0:N], func=AF.Square), reads=[yc], writes=[sq])
                p2 = nxt("pmm", pmm)
                S.op("pe", lambda e: e.matmul(p2[:, 0:N], bones, sq[:, 0:N], start=True, stop=True), reads=[cs_, sq], writes=[p2])
                rs = nxt("tmp", TMP)
                S.op("dve", lambda e: e.tensor_scalar(out=rs[:, 0:N], in0=p2[:, 0:N], scalar1=1.0 / 64, scalar2=GN_EPS, op0=ALU.mult, op1=ALU.add),
                     reads=[p2], writes=[rs])
                S.op("act", lambda e: e.activation(out=rs[:, 0:N], in_=rs[:, 0:N], func=AF.Sqrt), reads=[rs], writes=[rs])
                S.op("dve", lambda e: e.reciprocal(out=rs[:, 0:N], in_=rs[:, 0:N]), reads=[rs], writes=[rs])
                S.op("dve", lambda e: e.tensor_tensor(out=yc[:, 0:N], in0=yc[:, 0:N], in1=rs[:, 0:N], op=ALU.mult), reads=[yc, rs], writes=[yc])
                S.op("dve", lambda e: e.tensor_scalar(out=yc[:, 0:N], in0=yc[:, 0:N], scalar1=pv[:, PV2["lnxg"] + c:PV2["lnxg"] + c + 1],
                                                       scalar2=pv[:, PV2["lnxb"] + c:PV2["lnxb"] + c + 1], op0=ALU.mult, op1=ALU.add),
                     reads=[yc, pv], writes=[yc])
                S.op("pool", lambda e: e.tensor_tensor(out=yc[:, 0:N], in0=yc[:, 0:N], in1=bv[:, 0:N], op=ALU.add), reads=[yc, bv], writes=[yc])
                S.op("pool", lambda e: e.tensor_tensor(out=roT[:, c, 0:N], in0=yc[:, 0:N], in1=gm[:, 0:N], op=ALU.mult), reads=[yc, gm], writes=[roT])
            for c in range(4):
                z_ = load(FA["z"] + c, col0, N); x0 = load(FA["x0"] + c, col0, N); zc_ = load(FA["zc"] + c, col0, N)
                t_ = nxt("tmp", TMP)
                S.op("dve", lambda e: e.scalar_tensor_tensor(out=t_[:, 0:N], in0=z_[:, 0:N], scalar=pv[:, PV2["hb"] + c:PV2["hb"] + c + 1],
                                                             in1=zc_[:, 0:N], op0=ALU.mult, op1=ALU.add), reads=[z_, zc_, pv], writes=[t_])
                S.op("pool", lambda e: e.tensor_tensor(out=hoT[:, c, 0:N], in0=t_[:, 0:N], in1=x0[:, 0:N], op=ALU.mult), reads=[t_, x0], writes=[hoT])
            for oc in range(8):
                gr = load(FA["gates"] + oc, col0, N); gh = load(FA["gates"] + 8 + oc, col0, N)
                pr = nxt("pmm", pmm)
                for k in range(4):
                    S.op("pe", lambda e, k=k: e.matmul(pr[:, 0:N], wbr_b[:, k, oc * 128:(oc + 1) * 128], roT[:, k, 0:N], start=(k == 0), stop=(k == 3)),
                         reads=[wbr_b, roT], writes=[pr], inc=(k == 3))
                ph = nxt("pmm", pmm)
                for k in range(4):
                    S.op("pe", lambda e, k=k: e.matmul(ph[:, 0:N], wbh_b[:, k, oc * 128:(oc + 1) * 128], hoT[:, k, 0:N], start=(k == 0), stop=(k == 3)),
                         reads=[wbh_b, hoT], writes=[ph], inc=(k == 3))
                m1 = nxt("tmp", TMP)
                S.op("dve", lambda e: e.tensor_tensor(out=m1[:, 0:N], in0=pr[:, 0:N], in1=gr[:, 0:N], op=ALU.mult), reads=[pr, gr], writes=[m1])
                m2 = nxt("tmp", TMP)
                S.op("dve", lambda e: e.tensor_tensor(out=m2[:, 0:N], in0=ph[:, 0:N], in1=gh[:, 0:N], op=ALU.mult), reads=[ph, gh], writes=[m2])
                S.op("pool", lambda e: e.tensor_tensor(out=mT[:, oc, 0:N], in0=m1[:, 0:N], in1=m2[:, 0:N], op=ALU.add), reads=[m1, m2], writes=[mT])
            for t in range(N // 128):
                k2 = tcount[0] % 2; tcount[0] += 1
                tok0 = col0 + t * 128
                x_ = XT[k2]; t1 = T1[k2]; xo = XO[k2]; ht = HT[k2]; sm = SM[k2]; rt = RT[k2]; psr = PSr[k2]; gs4 = GS4[k2]
                S.dma("sp", x_[:], xres[tok0:tok0 + 128, :], reads=[d_in], writes=[x_])
                for hf in range(2):
                    pm_ = nxt("pmm", pmm)
                    for k in range(8):
                        S.op("pe", lambda e, k=k: e.matmul(pm_[:, :], mT[:, k, t * 128:(t + 1) * 128], wo_b[:, k, hf * 512:(hf + 1) * 512],
                                                           start=(k == 0), stop=(k == 7)), reads=[mT, wo_b], writes=[pm_], inc=(k == 7))
                    S.op("dve", lambda e: e.tensor_tensor(out=t1[:, hf * 512:(hf + 1) * 512], in0=pm_[:, :], in1=G2B[s][:, hf * 512:(hf + 1) * 512],
                                                          op=ALU.mult), reads=[pm_, G2B[s]], writes=[t1])
                S.op("dve", lambda e: e.scalar_tensor_tensor(out=t1[:], in0=x_[:], scalar=ALPHA, in1=t1[:], op0=ALU.mult, op1=ALU.add),
                     reads=[x_, t1], writes=[t1])
                S.op("dve", lambda e: e.reduce_sum(out=sm[:, 0:1], in_=t1[:], axis=AX.X), reads=[t1], writes=[sm])
                S.op("dve", lambda e: e.tensor_scalar(out=sm[:, 1:2], in0=sm[:, 0:1], scalar1=-1.0 / D, scalar2=None, op0=ALU.mult), reads=[sm], writes=[sm])
                S.op("dve", lambda e: e.tensor_scalar(out=t1[:], in0=t1[:], scalar1=sm[:, 1:2], scalar2=None, op0=ALU.add), reads=[t1, sm], writes=[t1])
                S.op("act", lambda e: e.activation(out=SQ[:], in_=t1[:], func=AF.Square), reads=[t1], writes=[SQ])
                S.op("dve", lambda e: e.reduce_sum(out=sm[:, 2:3], in_=SQ[:], axis=AX.X), reads=[SQ], writes=[sm])
                S.op("dve", lambda e: e.tensor_scalar(out=sm[:, 3:4], in0=sm[:, 2:3], scalar1=1.0 / D, scalar2=LN_EPS, op0=ALU.mult, op1=ALU.add),
                     reads=[sm], writes=[sm])
                S.op("act", lambda e: e.activation(out=sm[:, 3:4], in_=sm[:, 3:4], func=AF.Sqrt), reads=[sm], writes=[sm])
                S.op("dve", lambda e: e.reciprocal(out=sm[:, 4:5], in_=sm[:, 3:4]), reads=[sm], writes=[sm])
                S.op("dve", lambda e: e.scalar_tensor_tensor(out=xo[:], in0=t1[:], scalar=sm[:, 4:5], in1=LNG[:], op0=ALU.mult, op1=ALU.mult),
                     reads=[t1, sm, LNG], writes=[xo])
                S.op("pool", lambda e: e.tensor_tensor(out=xo[:], in0=xo[:], in1=LNB[:], op=ALU.add), reads=[xo, LNB], writes=[xo])
                outdma(xl1[tok0:tok0 + 128, :], xo, xo[:], q="act")
                for hh in range(2):
                    p_ = nxt("ptr", ptr)
                    for kk in range(4):
                        k = hh * 4 + kk
                        S.op("pe", lambda e, kk=kk, k=k: e.transpose(out=p_[:, kk, :], in_=xo[:, k * 128:(k + 1) * 128], identity=ident),
                             reads=[xo, cs_], writes=[p_], inc=(kk == 3))
                    for kk in range(4):
                        k = hh * 4 + kk
                        S.op("act", lambda e, kk=kk, k=k: e.activation(out=ht[:, k, :], in_=p_[:, kk, :], func=AF.Identity,
                                                                       scale=modv[:, 8 + k, s:s + 1], bias=modv[:, k, s:s + 1]),
                             reads=[p_, modv], writes=[ht])
                outdma(hT.rearrange("(k p) t -> p k t", p=128)[:, :, tok0:tok0 + 128], ht, ht[:], q="sp")
                for k in range(8):
                    S.op("pe", lambda e, k=k: e.matmul(P_RT[:, :], ht[:, k, :], wr[:, k, :], start=(k == 0), stop=(k == 7)), reads=[ht, wr], writes=[P_RT], inc=(k == 7))
                LG = rt[:, 0, :]; EX = rt[:, 1, :]; SC = rt[:, 2, :]; SEL = rt[:, 3, :]; ING = rt[:, 4, :]; MSK = rt[:, 5, :]
                IS1 = rt[:, 6, :]; MK2 = rt[:, 7, :]; IS2 = rt[:, 8, :]; WS = rt[:, 9, :]; PEN = rt[:, 10, :]

                def dv(fn, rd=(), wr_=()):
                    S.op("dve", fn, reads=list(rd), writes=list(wr_))
                dv(lambda e: e.tensor_copy(out=LG, in_=P_RT[:, :]), [P_RT], [rt])
                dv(lambda e: e.reduce_max(out=gs4[:, 0:1], in_=LG, axis=AX.X), [rt], [gs4])
                dv(lambda e: e.tensor_scalar(out=gs4[:, 1:2], in0=gs4[:, 0:1], scalar1=-1.0, scalar2=None, op0=ALU.mult), [gs4], [gs4])
                S.op("act", lambda e: e.activation(out=EX, in_=LG, func=AF.Exp, bias=gs4[:, 1:2], scale=1.0), reads=[rt, gs4], writes=[rt])
                dv(lambda e: e.reduce_sum(out=gs4[:, 2:3], in_=EX, axis=AX.X), [rt], [gs4])
                dv(lambda e: e.reciprocal(out=gs4[:, 3:4], in_=gs4[:, 2:3]), [gs4], [gs4])
                dv(lambda e: e.tensor_scalar(out=SC, in0=EX, scalar1=gs4[:, 3:4], scalar2=None, op0=ALU.mult), [rt, gs4], [rt])
                dv(lambda e: e.tensor_tensor(out=SEL, in0=SC, in1=RB[:], op=ALU.add), [rt, RB], [rt])
                selv = SEL.rearrange("p (g j) -> p g j", j=4)
                for pi, (a_, b_) in enumerate([(0, 1), (0, 2), (0, 3), (1, 2), (1, 3), (2, 3)]):
                    dv(lambda e, pi=pi, a_=a_, b_=b_: e.tensor_tensor(out=psr[:, :, pi], in0=selv[:, :, a_], in1=selv[:, :, b_], op=ALU.add), [rt], [psr])
                dv(lambda e: e.tensor_reduce(out=gs4[:, 4:8], in_=psr[:], axis=AX.X, op=ALU.max), [psr], [gs4])
                dv(lambda e: e.reduce_max(out=sm[:, 5:6], in_=gs4[:, 4:8], axis=AX.X), [gs4], [sm])
                dv(lambda e: e.tensor_scalar(out=gs4[:, 4:8], in0=gs4[:, 4:8], scalar1=sm[:, 5:6], scalar2=None, op0=ALU.is_equal), [gs4, sm], [gs4])
                ingv = ING.rearrange("p (g j) -> p g j", j=4)
                for j in range(4):
                    dv(lambda e, j=j: e.tensor_copy(out=ingv[:, :, j], in_=gs4[:, 4:8]), [gs4], [rt])
                dv(lambda e: e.tensor_tensor(out=MSK, in0=SEL, in1=ING, op=ALU.mult), [rt], [rt])
                dv(lambda e: e.tensor_scalar(out=PEN, in0=ING, scalar1=-1.0, scalar2=BIG, op0=ALU.add, op1=ALU.mult), [rt], [rt])
                dv(lambda e: e.tensor_tensor(out=MSK, in0=MSK, in1=PEN, op=ALU.add), [rt], [rt])
                dv(lambda e: e.reduce_max(out=sm[:, 6:7], in_=MSK, axis=AX.X), [rt], [sm])
                dv(lambda e: e.tensor_scalar(out=IS1, in0=MSK, scalar1=sm[:, 6:7], scalar2=None, op0=ALU.is_equal), [rt, sm], [rt])
                dv(lambda e: e.scalar_tensor_tensor(out=MK2, in0=IS1, scalar=-BIG, in1=MSK, op0=ALU.mult, op1=ALU.add), [rt], [rt])
                dv(lambda e: e.reduce_max(out=sm[:, 7:8], in_=MK2, axis=AX.X), [rt], [sm])
                dv(lambda e: e.tensor_scalar(out=IS2, in0=MK2, scalar1=sm[:, 7:8], scalar2=None, op0=ALU.is_equal), [rt, sm], [rt])
                dv(lambda e: e.tensor_tensor(out=IS1, in0=IS1, in1=IS2, op=ALU.add), [rt], [rt])
                dv(lambda e: e.tensor_tensor(out=WS, in0=SC, in1=IS1, op=ALU.mult), [rt], [rt])
                dv(lambda e: e.reduce_sum(out=gs4[:, 0:1], in_=WS, axis=AX.X), [rt], [gs4])
                dv(lambda e: e.reciprocal(out=gs4[:, 1:2], in_=gs4[:, 0:1]), [gs4], [gs4])
                dv(lambda e: e.tensor_scalar(out=WS, in0=WS, scalar1=gs4[:, 1:2], scalar2=None, op0=ALU.mult), [rt, gs4], [rt])
                outdma(wts[tok0:tok0 + 128, :], rt, WS, q="act")
        S.finish(outs)
    return nc


def kc1_consts():
    c = np.zeros((128, 512), np.float32)
    c[:, 0:128] = np.eye(128)
    c[0:64, 128:192] = 1.0; c[64:128, 192:256] = 1.0
    c[0, 256:384] = 1.0
    c[1, 384:512] = 1.0
    return c


def kc1_inputs(inp, l, fa, xres):
    def fm(vec, k):
        return np.asarray(vec, np.float32).reshape(k, 128).T
    pv = np.zeros((128, NPV2), np.float32)
    pv[:, 0:4] = fm(inp["rwkv_lnx_g"][l], 4); pv[:, 4:8] = fm(inp["rwkv_lnx_b"][l], 4); pv[:, 8:12] = fm(inp["hy_bias"][l], 4)
    pv[:, 12:60] = fm(inp["b_mod"][l], 48)
    rowp = np.zeros((4, D), np.float32)
    rowp[0] = inp["ln_g"][l, 0]; rowp[1] = inp["ln_b"][l, 0]; rowp[2] = inp["b_mod"][l][2048:3072]; rowp[3, 0:16] = inp["router_bias"]
    cc = np.concatenate([inp["c"][0].reshape(8, 128).T, inp["c_ctx"].reshape(8, 128).T], axis=1)
    return dict(fa=fa, xres=xres, cc=np.ascontiguousarray(cc, np.float32), wmod=np.ascontiguousarray(inp["w_mod"][l][:, 2048:5120]),
                pvec=pv, rowp=rowp, wbr=np.ascontiguousarray(inp["w_branch"][l].reshape(1024, 1024)), wout=inp["w_out"][l],
                wrt=inp["w_router"], cst=kc1_consts())


D = 1024
TO = 2304
NE = 16
ALPHA = 8 ** 0.25
LN_EPS = 1e-5
HALF = 1152


def build_kc2(ne=NE):
    nc = bass.Bass("TRN2", target_bir_lowering=False)
    hT = nc.dram_tensor("hT", [D, TO], F32, kind="ExternalInput").ap()
    xl1 = nc.dram_tensor("xl1", [TO, D], F32, kind="ExternalInput").ap()
    wts = nc.dram_tensor("wts", [TO, 16], F32, kind="ExternalInput").ap()
    wg = nc.dram_tensor("wg", [NE, D, D], BF16, kind="ExternalInput").ap()
    wu = nc.dram_tensor("wu", [NE, D, D], BF16, kind="ExternalInput").ap()
    wd = nc.dram_tensor("wd", [NE, D, D], BF16, kind="ExternalInput").ap()
    cc = nc.dram_tensor("cc", [128, 16], F32, kind="ExternalInput").ap()
    wmod = nc.dram_tensor("wmod", [D, D], F32, kind="ExternalInput").ap()
    rowp = nc.dram_tensor("rowp", [3, D], F32, kind="ExternalInput").ap()
    cst = nc.dram_tensor("cst", [2, 256], F32, kind="ExternalInput").ap()
    xl2 = nc.dram_tensor("xl2", [TO, D], F32, kind="ExternalOutput").ap()
    with contextlib.ExitStack() as st:
        S = Sched(nc, st)
        d_in = S.dram(hT)
        d_w = S.dram(wg)
        outs = []
        sel = S.sb([2, 256], F32, "sel"); S.dma("sp", sel[:], cst, reads=[d_in], writes=[sel])
        LNG = S.sb([128, D], F32, "lng"); LNB = S.sb([128, D], F32, "lnb"); BM5 = S.sb([128, D], F32, "bm5")
        for i, t_ in enumerate([LNG, LNB, BM5]):
            S.dma("sp", t_[:], AP(rowp.tensor, i * D, [[0, 128], [1, D]]), reads=[d_in], writes=[t_])
        sc = S.sb([128, 16], F32, "sc"); S.dma("sp", sc[:], cc, reads=[d_in], writes=[sc])
        scs = S.sb([128, 16], F32, "scs")
        S.op("act", lambda e: e.activation(out=scs[:], in_=sc[:], func=AF.Silu), reads=[sc], writes=[scs])
        scr = S.sb([128, 8, 2], F32, "scr")
        S.op("dve", lambda e: e.tensor_copy(out=scr[:, :, 0], in_=scs[:, 0:8]), reads=[scs], writes=[scr])
        S.op("dve", lambda e: e.tensor_copy(out=scr[:, :, 1], in_=scs[:, 8:16]), reads=[scs], writes=[scr])
        pgu = [S.ps([128, 512], F32, "pgu%d" % i) for i in range(4)]
        pdn = [S.ps([128, 512], F32, "pdn%d" % i) for i in range(2)]
        prow = S.ps([128, 512], F32, "prow")
        pbc = S.ps([128, 512], F32, "pbc")
        wmv = wmod.rearrange("(k p) n -> p k n", p=128)
        wm5 = S.sb([128, 8, 256], F32, "wm5")
        g5row = S.sb([2, D], F32, "g5row")
        G5B = [S.sb([128, D], F32, "g5b%d" % s) for s in range(2)]
        for q in range(4):
            S.dma("sp", wm5[:], wmv[:, :, q * 256:(q + 1) * 256], reads=[d_in], writes=[wm5])
            for k in range(8):
                S.op("pe", lambda e, k=k: e.matmul(prow[0:2, 0:256], scr[:, k, :], wm5[:, k, :], start=(k == 0), stop=(k == 7)),
                     reads=[scr, wm5], writes=[prow], inc=(k == 7))
            S.op("act", lambda e: e.copy(out=g5row[:, q * 256:(q + 1) * 256], in_=prow[0:2, 0:256]), reads=[prow], writes=[g5row])
        for s in range(2):
            for hf in range(2):
                S.op("pe", lambda e: e.matmul(pbc[:, :], sel[0:2, 128 * s:128 * s + 128], g5row[:, hf * 512:(hf + 1) * 512], start=True, stop=True),
                     reads=[sel, g5row], writes=[pbc])
                S.op("dve", lambda e: e.tensor_tensor(out=G5B[s][:, hf * 512:(hf + 1) * 512], in0=pbc[:, :], in1=BM5[:, hf * 512:(hf + 1) * 512],
                                                      op=ALU.add), reads=[pbc, BM5], writes=[G5B[s]])

        hTb = S.sb([128, 8, HALF], BF16, "hTb")
        WT = S.sb([128, 9, 16], F32, "wt")
        ACC = [S.sb([128, D], F32, "acc%d" % i) for i in range(9)]
        Wg = S.sb([128, 8, D], BF16, "Wg"); Wu = S.sb([128, 8, D], BF16, "Wu"); Wd = S.sb([128, 8, D], BF16, "Wd")
        HM = S.sb([128, 8, HALF], BF16, "hm")
        SG = [S.sb([128, 512], F32, "sg%d" % i) for i in range(2)]
        XT = S.sb([128, D], F32, "xt"); T1 = S.sb([128, D], F32, "t1"); SQ = S.sb([128, D], F32, "sq"); XO = S.sb([128, D], F32, "xo")
        SM = S.sb([128, 8], F32, "sm")
        cnt = {"pgu": 0, "pdn": 0, "sg": 0}

        def nxt(key, lst):
            i = cnt[key]; cnt[key] = (i + 1) % len(lst); return lst[i]
        hTv = hT.rearrange("(k p) t -> p k t", p=128)
        groups = [(0, 384), (384, 384), (768, 384)]

        wq = [0]

        def load_w(dst, src, e):
            q = "sp" if wq[0] % 2 == 0 else "act"
            wq[0] += 1
            S.dma(q, dst[:], src[e].rearrange("(k p) n -> p k n", p=128), reads=[d_w], writes=[dst])

        for ps_ in range(2):
            tok0 = ps_ * HALF
            S.dma("pool", hTb[:], hTv[:, :, tok0:tok0 + HALF], reads=[d_in], writes=[hTb])
            S.dma("sp", WT[:], wts[tok0:tok0 + HALF, :].rearrange("(t p) e -> p t e", p=128), reads=[d_in], writes=[WT])
            load_w(Wg, wg, 0); load_w(Wu, wu, 0); load_w(Wd, wd, 0)
            for e_ in range(ne):
                for (c0, N) in groups:
                    for j in range(8):
                        pg = nxt("pgu", pgu); pu = nxt("pgu", pgu)
                        for k in range(8):
                            S.op("pe", lambda e, k=k: e.matmul(pg[:, 0:N], Wg[:, k, j * 128:(j + 1) * 128], hTb[:, k, c0:c0 + N], start=(k == 0), stop=(k == 7)),
                                 reads=[Wg, hTb], writes=[pg], inc=(k == 7))
                        for k in range(8):
                            S.op("pe", lambda e, k=k: e.matmul(pu[:, 0:N], Wu[:, k, j * 128:(j + 1) * 128], hTb[:, k, c0:c0 + N], start=(k == 0), stop=(k == 7)),
                                 reads=[Wu, hTb], writes=[pu], inc=(k == 7))
                        sg = nxt("sg", SG)
                        S.op("act", lambda e: e.activation(out=sg[:, 0:N], in_=pg[:, 0:N], func=AF.Silu), reads=[pg], writes=[sg])
                        S.op("dve", lambda e: e.tensor_tensor(out=HM[:, j, c0:c0 + N], in0=pu[:, 0:N], in1=sg[:, 0:N], op=ALU.mult),
                             reads=[pu, sg], writes=[HM])
                if e_ + 1 < ne:
                    load_w(Wg, wg, e_ + 1); load_w(Wu, wu, e_ + 1)
                for t in range(9):
                    for hf in range(2):
                        pd = nxt("pdn", pdn)
                        for j in range(8):
                            S.op("pe", lambda e, j=j: e.matmul(pd[:, :], HM[:, j, t * 128:(t + 1) * 128], Wd[:, j, hf * 512:(hf + 1) * 512],
                                                               start=(j == 0), stop=(j == 7)), reads=[HM, Wd], writes=[pd], inc=(j == 7))
                        acc = ACC[t]
                        if e_ == 0:
                            S.op("dve", lambda e: e.tensor_scalar(out=acc[:, hf * 512:(hf + 1) * 512], in0=pd[:, :], scalar1=WT[:, t, e_:e_ + 1],
                                                                   scalar2=None, op0=ALU.mult), reads=[pd, WT], writes=[acc])
                        else:
                            S.op("dve", lambda e: e.scalar_tensor_tensor(out=acc[:, hf * 512:(hf + 1) * 512], in0=pd[:, :], scalar=WT[:, t, e_:e_ + 1],
                                                                         in1=acc[:, hf * 512:(hf + 1) * 512], op0=ALU.mult, op1=ALU.add),
                                 reads=[pd, WT, acc], writes=[acc])
                if e_ + 1 < ne:
                    load_w(Wd, wd, e_ + 1)
            for t in range(9):
                tk = tok0 + t * 128
                s = 1 if tk >= 2048 else 0
                acc = ACC[t]
                S.dma("sp", XT[:], xl1[tk:tk + 128, :], reads=[d_in], writes=[XT])
                S.op("pool", lambda e: e.tensor_tensor(out=T1[:], in0=acc[:], in1=G5B[s][:], op=ALU.mult), reads=[acc, G5B[s]], writes=[T1])
                S.op("dve", lambda e: e.scalar_tensor_tensor(out=T1[:], in0=XT[:], scalar=ALPHA, in1=T1[:], op0=ALU.mult, op1=ALU.add),
                     reads=[XT, T1], writes=[T1])
                S.op("dve", lambda e: e.reduce_sum(out=SM[:, 0:1], in_=T1[:], axis=AX.X), reads=[T1], writes=[SM])
                S.op("dve", lambda e: e.tensor_scalar(out=SM[:, 1:2], in0=SM[:, 0:1], scalar1=-1.0 / D, scalar2=None, op0=ALU.mult), reads=[SM], writes=[SM])
                S.op("dve", lambda e: e.tensor_scalar(out=T1[:], in0=T1[:], scalar1=SM[:, 1:2], scalar2=None, op0=ALU.add), reads=[T1, SM], writes=[T1])
                S.op("act", lambda e: e.activation(out=SQ[:], in_=T1[:], func=AF.Square), reads=[T1], writes=[SQ])
                S.op("dve", lambda e: e.reduce_sum(out=SM[:, 2:3], in_=SQ[:], axis=AX.X), reads=[SQ], writes=[SM])
                S.op("dve", lambda e: e.tensor_scalar(out=SM[:, 3:4], in0=SM[:, 2:3], scalar1=1.0 / D, scalar2=LN_EPS, op0=ALU.mult, op1=ALU.add),
                     reads=[SM], writes=[SM])
                S.op("act", lambda e: e.activation(out=SM[:, 3:4], in_=SM[:, 3:4], func=AF.Sqrt), reads=[SM], writes=[SM])
                S.op("dve", lambda e: e.reciprocal(out=SM[:, 4:5], in_=SM[:, 3:4]), reads=[SM], writes=[SM])
                S.op("dve", lambda e: e.scalar_tensor_tensor(out=XO[:], in0=T1[:], scalar=SM[:, 4:5], in1=LNG[:], op0=ALU.mult, op1=ALU.mult),
                     reads=[T1, SM, LNG], writes=[XO])
                S.op("pool", lambda e: e.tensor_tensor(out=XO[:], in0=XO[:], in1=LNB[:], op=ALU.add), reads=[XO, LNB], writes=[XO])
                ob = S.dram(xl2); outs.append(ob)
                S.dma("act", xl2[tk:tk + 128, :], XO[:], reads=[XO], writes=[ob])
        S.finish(outs)
    return nc


def kc2_inputs(inp, l, hT, xl1, wts, wgb, wub, wdb):
    rowp = np.zeros((3, D), np.float32)
    rowp[0] = inp["ln_g"][l, 1]; rowp[1] = inp["ln_b"][l, 1]; rowp[2] = inp["b_mod"][l][5120:6144]
    cc = np.concatenate([inp["c"][0].reshape(8, 128).T, inp["c_ctx"].reshape(8, 128).T], axis=1)
    sel = np.zeros((2, 256), np.float32)
    sel[0, 0:128] = 1.0; sel[1, 128:256] = 1.0
    return dict(hT=hT, xl1=xl1, wts=wts, wg=wgb, wu=wub, wd=wdb,
                cc=np.ascontiguousarray(cc, np.float32), wmod=np.ascontiguousarray(inp["w_mod"][l][:, 5120:6144]), rowp=rowp, cst=sel)


NMAT = 24


def build_kw(nmat=NMAT):
    nc = bass.Bass("TRN2", target_bir_lowering=False)
    w = nc.dram_tensor("w", [nmat, 1024, 1024], F32, kind="ExternalInput").ap()
    o = nc.dram_tensor("o", [nmat, 1024, 1024], BF16, kind="ExternalOutput").ap()
    with contextlib.ExitStack() as st:
        S = Sched(nc, st)
        d_in = S.dram(w)
        outs = []
        A = [S.sb([128, 8, 1024], F32, "a%d" % i) for i in range(2)]
        Bt = [S.sb([128, 8, 1024], BF16, "b%d" % i) for i in range(2)]
        for m in range(nmat):
            a, b = A[m % 2], Bt[m % 2]
            S.dma("sp", a[:], w[m].rearrange("(k p) n -> p k n", p=128), reads=[d_in], writes=[a])
            S.op("act", lambda e: e.copy(out=b[:, 0:3, :], in_=a[:, 0:3, :]), reads=[a], writes=[b])
            S.op("dve", lambda e: e.tensor_copy(out=b[:, 3:6, :], in_=a[:, 3:6, :]), reads=[a], writes=[b])
            S.op("pool", lambda e: e.tensor_copy(out=b[:, 6:8, :], in_=a[:, 6:8, :]), reads=[a], writes=[b])
            ob = S.dram(o); outs.append(ob)
            S.dma("act", o[m].rearrange("(k p) n -> p k n", p=128), b[:], reads=[b], writes=[ob])
        S.finish(outs)
    return nc


_PROGS = {}


def _prog(name, fn):
    if name not in _PROGS:
        _PROGS[name] = fn()
    return _PROGS[name]


def _run(nc, in_maps):
    res = run_bass_kernel_spmd(nc, in_maps, core_ids=list(range(len(in_maps))))
    return res.results


def _rows(name, n=4):
    b = OUT_BASE[name] * 128
    return slice(b, b + n * 128)


def _layer(inp, l, xl, xc):
    C8 = range(8)
    oa = [r["out"] for r in _run(_prog("ka", build_ka), [ka_inputs(inp, l, xl, xc, c) for c in C8])]

    def full(name, n=4):
        lat = np.concatenate([oa[c][_rows(name, n), 0:2048] for c in C8], axis=1)
        return lat, oa[0][_rows(name, n), 2048:2304]
    fr, fv_, fnkk = full("r"), full("v"), full("nkk")
    ys = []
    consts = kb1_consts()
    for d, (lwn, kdn, kbn) in enumerate([("lwf", "kdf", "kbf"), ("lwb", "kdb", "kbb")]):
        flw, fkd, fkb = full(lwn), full(kdn), full(kbn)
        ims = []
        for h in C8:
            hs = slice(64 * h, 64 * h + 64)
            rows = []
            for (lat, ctx) in (fr, fv_, fnkk, flw, fkd, fkb):
                a, b = lat[hs], ctx[hs]
                if d == 1:
                    a, b = a[:, ::-1], b[:, ::-1]
                rows.append(np.concatenate([b, a], axis=1))
            im = dict(sin0=np.ascontiguousarray(np.concatenate(rows, axis=0), np.float32))
            im.update(consts)
            ims.append(im)
        yo = [r["y0"] for r in _run(_prog("kb1", build_kb1), ims)]
        ylat = np.concatenate([y[:, 256:] for y in yo], axis=0)
        yctx = np.concatenate([y[:, :256] for y in yo], axis=0)
        if d == 1:
            ylat, yctx = ylat[:, ::-1], yctx[:, ::-1]
        ys.append((ylat, yctx))
    z_l, z_c = full("z")
    ob = _run(_prog("kb2", build_kb2), [kb2_inputs(inp, l, c, z_l, z_c) for c in C8])
    zc_l = np.concatenate([r["yl"] for r in ob], axis=0)
    zc_c = np.concatenate([r["yc"] for r in ob], axis=0)
    ims = []
    for c in C8:
        sl = slice(2048 * c, 2048 * c + 2048)
        fa = np.empty((FA_CH * 128, TO), np.float32)

        def put(name, arr):
            fa[FA[name] * 128:FA[name] * 128 + arr.shape[0]] = arr
        put("yf", np.concatenate([ys[0][0][:, sl], ys[0][1]], axis=1))
        put("yb", np.concatenate([ys[1][0][:, sl], ys[1][1]], axis=1))
        for nm in ("bv", "gmul", "z", "x0"):
            put(nm, oa[c][_rows(nm)])
        put("zc", np.concatenate([zc_l[:, sl], zc_c], axis=1))
        put("gates", oa[c][_rows("gates", 16)])
        xres = np.ascontiguousarray(np.concatenate([xl[sl], xc], axis=0), np.float32)
        ims.append(kc1_inputs(inp, l, fa, xres))
    oc1 = _run(_prog("kc1", build_kc1), ims)
    del oa, ims
    oc2 = _run(_prog("kc2", build_kc2), [kc2_inputs(inp, l, r["hT"], r["xl1"], r["wts"], inp["_wb"][(l, "w_gate")], inp["_wb"][(l, "w_up")], inp["_wb"][(l, "w_down")]) for r in oc1])
    xl_n = np.ascontiguousarray(np.concatenate([r["xl2"][0:2048] for r in oc2], axis=0), np.float32)
    xc_n = np.ascontiguousarray(oc2[0]["xl2"][2048:2304], np.float32)
    return xl_n, xc_n


def _cast_weights(inp):
    ims = []
    for c in range(8):
        mats = []
        for l in range(4):
            for e2 in range(2):
                for nm in ("w_gate", "w_up", "w_down"):
                    mats.append(inp[nm][l][2 * c + e2])
        ims.append(dict(w=np.ascontiguousarray(np.stack(mats, axis=0), np.float32)))
    outs = [r["o"] for r in _run(_prog("kw", build_kw), ims)]
    wb = {}
    for l in range(4):
        for k, nm in enumerate(("w_gate", "w_up", "w_down")):
            wb[(l, nm)] = np.stack([outs[e // 2][l * 6 + (e % 2) * 3 + k] for e in range(16)], axis=0)
    return wb


def kernel(**inputs):
    inp = {k: np.asarray(v) for k, v in inputs.items()}
    inp["_wb"] = _cast_weights(inp)
    xl = np.ascontiguousarray(inp["x"][0], np.float32)
    xc = np.ascontiguousarray(inp["ctx"][0], np.float32)
    for l in range(4):
        xl, xc = _layer(inp, l, xl, xc)
    return xl[None].astype(np.float32)
```
